# Optimizing a Trainium2 kernel written in Bass

```python
import math
import jax
import jax.numpy as jnp
from jax import lax
import numpy as np

D_MODEL = 1024
BATCH = 16
SEQ = 4096
DEPTH = 2

GRID_W = 64
CTX_LEN = 256
N_MOD = 6
EPS = 1e-6

MLA_HEADS = 8
MLA_NOPE = 64
MLA_ROPE = 32
MLA_V = 64
MLA_Q_LORA = 256
MLA_KV_LORA = 128
ROPE_BASE = 10000.0
Q_BLOCK = 128

CONV_CH = 256
CONV_K = 31

HY_CH = 256
HY_ORDER = 2
HY_SHORT_K = 3
HY_EMB = 33
HY_BANDS = 16
HY_HID = 64
HY_DECAY_SLOW = 3.07
HY_DECAY_FAST = 15.35

MLA_OUT = MLA_HEADS * MLA_V
MIX_WIDTH = MLA_OUT + CONV_CH + HY_CH
OFF_KV = MLA_Q_LORA
OFF_CONV = OFF_KV + MLA_KV_LORA + MLA_ROPE
OFF_HY = OFF_CONV + 2 * CONV_CH
IN_COLS = OFF_HY + (HY_ORDER + 1) * HY_CH

N_EXPERTS = 16
EXPERT_FF = 1024
CAPACITY_FACTOR = 2

kernel_name = 'hybrid_mla_conformer_hyena_ec_block'


def rmsnorm(x, g):
    xf = x.astype(jnp.float32)
    y = xf * lax.rsqrt(jnp.mean(xf * xf, axis=-1, keepdims=True) + EPS)
    return (y * g.astype(jnp.float32)).astype(x.dtype)


def layernorm(x, g, b):
    xf = x.astype(jnp.float32)
    mu = jnp.mean(xf, axis=-1, keepdims=True)
    var = jnp.mean(jnp.square(xf - mu), axis=-1, keepdims=True)
    y = (xf - mu) * lax.rsqrt(var + EPS)
    return (y * g.astype(jnp.float32) + b.astype(jnp.float32)).astype(x.dtype)


def modulate(h, shift, scale):
    return h * (1.0 + scale) + shift


def dwconv(x, w, b):
    ch = x.shape[-1]
    y = lax.conv_general_dilated(
        x, w[:, None, :].astype(x.dtype), window_strides=(1,), padding='SAME',
        dimension_numbers=('NWC', 'WIO', 'NWC'), feature_group_count=ch)
    return y + b.astype(x.dtype)


def axial_rope_tables(n_tok, dtype):
    rows = n_tok // GRID_W
    row = jnp.repeat(jnp.arange(rows, dtype=jnp.float32), GRID_W)
    col = jnp.tile(jnp.arange(GRID_W, dtype=jnp.float32), rows)
    half = MLA_ROPE // 2
    inv = ROPE_BASE ** (-jnp.arange(0, half, 2, dtype=jnp.float32) / half)
    ang = jnp.stack([row[:, None] * inv, col[:, None] * inv], axis=1)
    return jnp.cos(ang).astype(dtype), jnp.sin(ang).astype(dtype)


def apply_axial_rope(x, cos, sin):
    xs = x.reshape(x.shape[:-1] + (2, 2, MLA_ROPE // 4))
    x1, x2 = xs[..., 0, :], xs[..., 1, :]
    out = jnp.stack([x1 * cos - x2 * sin, x1 * sin + x2 * cos], axis=-2)
    return out.reshape(x.shape)


def mla_queries(u_q, p, cos, sin):
    bsz, n_tok, _ = u_q.shape
    q = (rmsnorm(u_q, p['q_a_g']) @ p['w_q_b']).reshape(bsz, n_tok, MLA_HEADS, MLA_NOPE + MLA_ROPE)
    q_nope, q_rope = q[..., :MLA_NOPE], q[..., MLA_NOPE:]
    if cos is not None:
        q_rope = apply_axial_rope(q_rope, cos[:, None], sin[:, None])
    return q_nope, q_rope


def mla_keys_values(u_kv, p, cos, sin):
    bsz, n_tok, _ = u_kv.shape
    kv = (rmsnorm(u_kv[..., :MLA_KV_LORA], p['kv_a_g']) @ p['w_kv_b']).reshape(
        bsz, n_tok, MLA_HEADS, MLA_NOPE + MLA_V)
    k_rope = u_kv[..., MLA_KV_LORA:]
    if cos is not None:
        k_rope = apply_axial_rope(k_rope, cos, sin)
    return kv[..., :MLA_NOPE], k_rope, kv[..., MLA_NOPE:]


def mla_attend(q_nope, q_rope, k_nope, k_rope, v):
    bsz, n_q, n_h, _ = q_nope.shape
    qb = min(Q_BLOCK, n_q)
    nb = n_q // qb
    scale = (MLA_NOPE + MLA_ROPE) ** -0.5

    def blocks(t):
        return jnp.moveaxis(t.reshape((bsz, nb, qb) + t.shape[2:]), 1, 0)

    def attend_block(args):
        qn, qr = args
        s = (jnp.einsum('bqhd,bkhd->bhqk', qn, k_nope, preferred_element_type=jnp.float32)
             + jnp.einsum('bqhr,bkr->bhqk', qr, k_rope, preferred_element_type=jnp.float32))
        w = jax.nn.softmax(s * scale, axis=-1).astype(v.dtype)
        return jnp.einsum('bhqk,bkhd->bqhd', w, v)

    o = lax.map(attend_block, (blocks(q_nope), blocks(q_rope)))
    return jnp.moveaxis(o, 0, 1).reshape(bsz, n_q, n_h * MLA_V)


def conformer_conv(u, p):
    a, g = u[..., :CONV_CH], u[..., CONV_CH:]
    y = a * jax.nn.sigmoid(g)
    y = dwconv(y, p['conv_dw_w'], p['conv_dw_b'])
    y = layernorm(y, p['conv_ln_g'], p['conv_ln_b'])
    return jax.nn.silu(y)


def hyena_filters(n_tok, p):
    f32 = jnp.float32
    t = jnp.linspace(0.0, 1.0, n_tok, dtype=f32)[:, None]
    w = 2.0 * math.pi * jnp.arange(n_tok, dtype=f32) / n_tok
    f = jnp.linspace(1e-4, HY_BANDS - 1, HY_BANDS, dtype=f32)
    fw = w[:, None] * f[None, :]
    z = jnp.concatenate([t, jnp.cos(fw), -jnp.sin(fw)], axis=-1)
    freq = p['hy_sin_freq'].astype(f32)
    h = jnp.sin(freq[0] * (z @ p['hy_w1'].astype(f32) + p['hy_b1'].astype(f32)))
    h = jnp.sin(freq[1] * (h @ p['hy_w2'].astype(f32) + p['hy_b2'].astype(f32)))
    h = (h @ p['hy_w3'].astype(f32)).reshape(n_tok, HY_ORDER, 2, HY_CH)
    decay = jnp.exp(-t * jnp.abs(p['hy_decay'].astype(f32)))
    h = h * decay[:, None, None, :]
    h_fwd, h_bwd = h[:, :, 0], h[:, :, 1]
    g = jnp.concatenate([h_fwd, jnp.zeros((1, HY_ORDER, HY_CH), f32), h_bwd[:0:-1]], axis=0)
    return g * lax.rsqrt(jnp.sum(g * g, axis=0, keepdims=True) + EPS)


def fft_long_conv(u, g_freq, skip):
    n_tok = u.shape[1]
    uf = u.astype(jnp.float32)
    spec = jnp.fft.rfft(uf, n=2 * n_tok, axis=1)
    y = jnp.fft.irfft(spec * g_freq[None], n=2 * n_tok, axis=1)[:, :n_tok]
    return (y + uf * skip.astype(jnp.float32)).astype(u.dtype)


def hyena(u, p):
    n_tok = u.shape[1]
    z = dwconv(u, p['hy_short_w'], p['hy_short_b'])
    v, x1, x2 = z[..., :HY_CH], z[..., HY_CH:2 * HY_CH], z[..., 2 * HY_CH:]
    g_freq = jnp.fft.rfft(hyena_filters(n_tok, p), n=2 * n_tok, axis=0)
    y = v
    for o, gate in enumerate((x1, x2)):
        y = gate * fft_long_conv(y, g_freq[:, o], p['hy_skip'][o])
    return y


def token_mixers(u, p, cos, sin, keys):
    q_nope, q_rope = mla_queries(u[..., :OFF_KV], p, cos, sin)
    att = mla_attend(q_nope, q_rope, *keys)
    conv = conformer_conv(u[..., OFF_CONV:OFF_HY], p)
    hy = hyena(u[..., OFF_HY:], p)
    gn = p['group_norm_g']
    y = jnp.concatenate([rmsnorm(att, gn[:MLA_OUT]),
                         rmsnorm(conv, gn[MLA_OUT:MLA_OUT + CONV_CH]),
                         rmsnorm(hy, gn[MLA_OUT + CONV_CH:])], axis=-1)
    return y @ p['w_out']


def expert_choice_ffn(h, p):
    bsz, n_tok, _ = h.shape
    cap = CAPACITY_FACTOR * n_tok // N_EXPERTS
    logits = jnp.einsum('bld,de->ble', h, p['router_w']).astype(jnp.float32)
    aff = jax.nn.softmax(logits, axis=-1)
    gates, idx = lax.top_k(jnp.swapaxes(aff, 1, 2), cap)
    bidx = jnp.arange(bsz)[:, None, None]
    xg = h[bidx, idx]
    a = jnp.einsum('becd,edf->becf', xg, p['w_gate'])
    b = jnp.einsum('becd,edf->becf', xg, p['w_up'])
    y = jnp.einsum('becf,efd->becd', jax.nn.silu(a) * b, p['w_down'])
    y = y * gates[..., None].astype(h.dtype)
    return jnp.zeros_like(h).at[bidx, idx].add(y)


def trunk_layer(xl, xc, c, c_ctx, p, cos, sin, last):
    mod_l = (jax.nn.silu(c) @ p['mod_w'] + p['mod_b'])[:, None, :]
    mod_c = jax.nn.silu(c_ctx) @ p['mod_w'] + p['mod_b']
    sh1_l, sc1_l, g1_l, sh2_l, sc2_l, g2_l = jnp.split(mod_l, N_MOD, axis=-1)
    sh1_c, sc1_c, g1_c, sh2_c, sc2_c, g2_c = jnp.split(mod_c, N_MOD, axis=-1)

    hc = modulate(rmsnorm(xc, p['norm1_g']), sh1_c, sc1_c)
    if last:
        uc_kv = hc @ p['w_in'][:, OFF_KV:OFF_CONV]
    else:
        uc = hc @ p['w_in']
        uc_kv = uc[..., OFF_KV:OFF_CONV]
    kv_c = mla_keys_values(uc_kv, p, None, None)

    hl = modulate(rmsnorm(xl, p['norm1_g']), sh1_l, sc1_l)
    ul = hl @ p['w_in']
    kv_l = mla_keys_values(ul[..., OFF_KV:OFF_CONV], p, cos, sin)
    keys_l = (jnp.concatenate([kv_l[0], kv_c[0]], axis=1),
              jnp.concatenate([kv_l[1], kv_c[1]], axis=1),
              jnp.concatenate([kv_l[2], kv_c[2]], axis=1))
    xl = xl + g1_l * token_mixers(ul, p, cos, sin, keys_l)
    hl2 = modulate(rmsnorm(xl, p['norm2_g']), sh2_l, sc2_l)
    xl = xl + g2_l * expert_choice_ffn(hl2, p)
    if last:
        return xl, None

    xc = xc + g1_c * token_mixers(uc, p, None, None, kv_c)
    hc2 = modulate(rmsnorm(xc, p['norm2_g']), sh2_c, sc2_c)
    xc = xc + g2_c * expert_choice_ffn(hc2, p)
    return xl, xc


def setup_inputs(seed: int = 0) -> dict:
    key = jax.random.key(seed)
    ks = iter(jax.random.split(key, 40))

    def nrm(shape, scale):
        return jax.random.normal(next(ks), shape, jnp.float32) * scale

    def gain(shape):
        return 1.0 + nrm(shape, 0.02)

    L = DEPTH
    hy_decay = jnp.linspace(HY_DECAY_SLOW, HY_DECAY_FAST, HY_CH, dtype=jnp.float32)[None, :] + nrm((L, HY_CH), 0.1)
    return {
        'x': nrm((BATCH, SEQ, D_MODEL), 1.0),
        'c': nrm((BATCH, D_MODEL), 1.0),
        'ctx': nrm((BATCH, CTX_LEN, D_MODEL), 1.0),
        'c_ctx': nrm((D_MODEL,), 1.0),
        'mod_w': nrm((L, D_MODEL, N_MOD * D_MODEL), 0.5 * D_MODEL ** -0.5),
        'mod_b': nrm((L, N_MOD * D_MODEL), 0.01),
        'norm1_g': gain((L, D_MODEL)),
        'w_in': nrm((L, D_MODEL, IN_COLS), D_MODEL ** -0.5),
        'q_a_g': gain((L, MLA_Q_LORA)),
        'w_q_b': nrm((L, MLA_Q_LORA, MLA_HEADS * (MLA_NOPE + MLA_ROPE)), MLA_Q_LORA ** -0.5),
        'kv_a_g': gain((L, MLA_KV_LORA)),
        'w_kv_b': nrm((L, MLA_KV_LORA, MLA_HEADS * (MLA_NOPE + MLA_V)), MLA_KV_LORA ** -0.5),
        'conv_dw_w': nrm((L, CONV_K, CONV_CH), CONV_K ** -0.5),
        'conv_dw_b': nrm((L, CONV_CH), 0.01),
        'conv_ln_g': gain((L, CONV_CH)),
        'conv_ln_b': nrm((L, CONV_CH), 0.01),
        'hy_short_w': nrm((L, HY_SHORT_K, (HY_ORDER + 1) * HY_CH), HY_SHORT_K ** -0.5),
        'hy_short_b': nrm((L, (HY_ORDER + 1) * HY_CH), 0.01),
        'hy_w1': nrm((L, HY_EMB, HY_HID), HY_EMB ** -0.5),
        'hy_b1': nrm((L, HY_HID), 0.1),
        'hy_w2': nrm((L, HY_HID, HY_HID), HY_HID ** -0.5),
        'hy_b2': nrm((L, HY_HID), 0.1),
        'hy_w3': nrm((L, HY_HID, HY_ORDER * 2 * HY_CH), HY_HID ** -0.5),
        'hy_sin_freq': 1.0 + nrm((L, 2, HY_HID), 0.1),
        'hy_decay': hy_decay,
        'hy_skip': nrm((L, HY_ORDER, HY_CH), 0.5),
        'group_norm_g': gain((L, MIX_WIDTH)),
        'w_out': nrm((L, MIX_WIDTH, D_MODEL), MIX_WIDTH ** -0.5),
        'norm2_g': gain((L, D_MODEL)),
        'router_w': nrm((L, D_MODEL, N_EXPERTS), D_MODEL ** -0.5),
        'w_gate': nrm((L, N_EXPERTS, D_MODEL, EXPERT_FF), D_MODEL ** -0.5),
        'w_up': nrm((L, N_EXPERTS, D_MODEL, EXPERT_FF), D_MODEL ** -0.5),
        'w_down': nrm((L, N_EXPERTS, EXPERT_FF, D_MODEL), EXPERT_FF ** -0.5),
        'final_norm_g': gain((D_MODEL,)),
    }


def reference(x, c, ctx, c_ctx, mod_w, mod_b, norm1_g, w_in, q_a_g, w_q_b, kv_a_g, w_kv_b,
              conv_dw_w, conv_dw_b, conv_ln_g, conv_ln_b, hy_short_w, hy_short_b,
              hy_w1, hy_b1, hy_w2, hy_b2, hy_w3, hy_sin_freq, hy_decay, hy_skip,
              group_norm_g, w_out, norm2_g, router_w, w_gate, w_up, w_down, final_norm_g):
    cos, sin = axial_rope_tables(x.shape[1], x.dtype)
    xl, xc = x, ctx
    for i in range(DEPTH):
        p = {
            'mod_w': mod_w[i], 'mod_b': mod_b[i], 'norm1_g': norm1_g[i], 'w_in': w_in[i],
            'q_a_g': q_a_g[i], 'w_q_b': w_q_b[i], 'kv_a_g': kv_a_g[i], 'w_kv_b': w_kv_b[i],
            'conv_dw_w': conv_dw_w[i], 'conv_dw_b': conv_dw_b[i],
            'conv_ln_g': conv_ln_g[i], 'conv_ln_b': conv_ln_b[i],
            'hy_short_w': hy_short_w[i], 'hy_short_b': hy_short_b[i],
            'hy_w1': hy_w1[i], 'hy_b1': hy_b1[i], 'hy_w2': hy_w2[i], 'hy_b2': hy_b2[i],
            'hy_w3': hy_w3[i], 'hy_sin_freq': hy_sin_freq[i], 'hy_decay': hy_decay[i],
            'hy_skip': hy_skip[i], 'group_norm_g': group_norm_g[i], 'w_out': w_out[i],
            'norm2_g': norm2_g[i], 'router_w': router_w[i],
            'w_gate': w_gate[i], 'w_up': w_up[i], 'w_down': w_down[i],
        }
        xl, xc = trunk_layer(xl, xc, c, c_ctx, p, cos, sin, i == DEPTH - 1)
    return rmsnorm(xl, final_norm_g)
```

```python
import math
import sys
from contextlib import ExitStack
import numpy as np
import ml_dtypes
import concourse.bass as bass
import concourse.mybir as mybir
from concourse.bass_utils import run_bass_kernel_spmd

F32 = mybir.dt.float32
BF16 = mybir.dt.bfloat16
AF = mybir.ActivationFunctionType
ALU = mybir.AluOpType
AX = mybir.AxisListType

SAME_ENGINE_SYNC = True
NDMA_SLOTS = 24

D = 1024
NH = 8
EPS = 1e-6
N_EXP = 16
GRID_W = 64
OFF_KV = 256
OFF_KR = 384
OFF_CONV = 416
OFF_HY = 928
IN_COLS = 1696
CONV_K = 31


class Res:
    __slots__ = ("name", "w", "r")

    def __init__(self, name=""):
        self.name = name
        self.w = None
        self.r = []


class Op:
    __slots__ = ("eng", "fn", "deps", "is_dma", "signals", "sem", "sigval", "clock", "waits", "line")

    def __init__(self, eng, fn, is_dma):
        f = sys._getframe(2)
        self.line = (f.f_lineno, f.f_back.f_lineno if f.f_back else 0)
        self.eng = eng
        self.fn = fn
        self.is_dma = is_dma
        self.deps = []
        self.signals = is_dma
        self.sem = None
        self.sigval = 0
        self.clock = None
        self.waits = []


class Sched:
    ENGS = ("pe", "act", "dve", "pool", "sp")

    def __init__(self, nc):
        self.nc = nc
        self.ops = []
        self.last = {}
        self.pending_dma = []
        self.reg_vals = set()
        self.regs = {}

    def _track(self, op, reads, writes):
        deps = []
        for r in reads:
            if r.w is not None:
                deps.append(r.w)
            if op.is_dma:
                r.r.append(op)
            else:
                r.r = [x for x in r.r if x.is_dma or x.eng != op.eng]
                r.r.append(op)
        for w in writes:
            if w.w is not None:
                deps.append(w.w)
            deps.extend(w.r)
            w.w = op
            w.r = []
        seen = set()
        for d in deps:
            if d is op or id(d) in seen:
                continue
            seen.add(id(d))
            op.deps.append(d)

    def op(self, eng, fn, reads=(), writes=()):
        o = Op(eng, fn, False)
        self._track(o, reads, writes)
        self.ops.append(o)
        self.last[eng] = o
        return o

    def dma(self, eng, fn, reads=(), writes=()):
        o = Op(eng, fn, True)
        self._track(o, reads, writes)
        self.ops.append(o)
        self.pending_dma.append(o)
        return o

    def barrier(self):
        deps = [o for o in self.last.values()] + list(self.pending_dma)
        self.pending_dma = []
        for e in self.ENGS:
            o = Op(e, None, False)
            o.deps = [d for d in deps]
            self.ops.append(o)

    def emit(self, stack):
        nc = self.nc
        for o in self.ops:
            for d in o.deps:
                if d.is_dma:
                    continue
                if d.eng != o.eng or o.is_dma:
                    d.signals = True
                elif SAME_ENGINE_SYNC and o.eng != "pe":
                    d.signals = True
        esem = {e: stack.enter_context(nc.semaphore("s_" + e)) for e in self.ENGS}
        ecount = {e: 0 for e in self.ENGS}
        clock = {e: {} for e in self.ENGS}
        slot_last = {}
        dcount = {e: 0 for e in self.ENGS}
        dsem = {}
        for e in sorted({o.eng for o in self.ops if o.is_dma}):
            dsem[e] = [stack.enter_context(nc.semaphore("d_%s_%d" % (e, i))) for i in range(NDMA_SLOTS)]
        per_eng = {e: [] for e in self.ENGS}
        nwaits = 0
        for o in self.ops:
            E = o.eng
            ck = clock[E]

            def need(d):
                nonlocal nwaits
                key = d.sem.num if d.is_dma else d.eng
                if not d.is_dma and d.eng == E and not o.is_dma:
                    if E == "pe" or not SAME_ENGINE_SYNC:
                        return
                if ck.get(key, 0) >= d.sigval:
                    return
                o.waits.append((d.sem, d.sigval))
                nwaits += 1
                for k, v in d.clock.items():
                    if ck.get(k, 0) < v:
                        ck[k] = v

            for d in o.deps:
                need(d)
            if o.is_dma:
                i = dcount[E]
                dcount[E] += 1
                s = i % NDMA_SLOTS
                prev = slot_last.get((E, s))
                if prev is not None:
                    need(prev)
                o.sem = dsem[E][s]
                o.sigval = (prev.sigval if prev is not None else 0) + 16
                slot_last[(E, s)] = o
                o.clock = dict(ck)
                o.clock[o.sem.num] = o.sigval
            else:
                o.sem = esem[E]
                if o.signals and o.fn is not None:
                    ecount[E] += 1
                o.sigval = ecount[E]
                o.clock = dict(ck)
                o.clock[E] = o.sigval
            per_eng[E].append(o)
        self.stats = dict(n_ops=len(self.ops), n_waits=nwaits, counts=dict(ecount),
                          per_eng={e: len(v) for e, v in per_eng.items()})
        final_waits = []
        for e in self.ENGS:
            if ecount[e] > 0:
                final_waits.append((esem[e], ecount[e]))
        for (e, s), o in slot_last.items():
            final_waits.append((o.sem, o.sigval))
        block = stack.enter_context(nc.Block())
        engobj = {"pe": "tensor", "act": "scalar", "dve": "vector", "pool": "gpsimd", "sp": "sync"}

        def make(e):
            lst = per_eng[e]

            def body(eng):
                if e == "pool":
                    for val in sorted(self.reg_vals):
                        r = eng.alloc_register("bc%d" % val)
                        eng.reg_mov(r, val)
                        self.regs[val] = r
                for o in lst:
                    for (sem, val) in o.waits:
                        eng.wait_ge(sem, val)
                    if o.fn is None:
                        continue
                    try:
                        ins = o.fn(eng)
                    except Exception:
                        print("EMIT FAIL at op created at lines", o.line, "eng", o.eng)
                        import traceback; traceback.print_exc()
                        raise
                    if o.is_dma:
                        ins.then_inc(o.sem, 16)
                    elif o.signals:
                        ins.then_inc(o.sem, 1)
                if e == "sp":
                    for (sem, val) in final_waits:
                        eng.wait_ge(sem, val)
            return body

        for e in self.ENGS:
            getattr(block, engobj[e])(make(e))


def _bf(a):
    return np.ascontiguousarray(a.astype(ml_dtypes.bfloat16))


def rope_tables(T):
    rows = T // GRID_W
    row = np.repeat(np.arange(rows, dtype=np.float32), GRID_W)
    col = np.tile(np.arange(GRID_W, dtype=np.float32), rows)
    half = 16
    inv = (10000.0 ** (-np.arange(0, half, 2, dtype=np.float32) / half)).astype(np.float32)
    ar = (row[:, None] * inv).astype(np.float32)
    ac = (col[:, None] * inv).astype(np.float32)
    cos = np.concatenate([np.cos(ar), np.cos(ar), np.cos(ac), np.cos(ac)], axis=1)
    sin = np.concatenate([-np.sin(ar), np.sin(ar), -np.sin(ac), np.sin(ac)], axis=1)
    return np.ascontiguousarray(cos.T.astype(np.float32)), np.ascontiguousarray(sin.T.astype(np.float32))


def hyena_consts(T):
    N = 2 * T
    t = np.linspace(0.0, 1.0, T, dtype=np.float32)
    w = (2.0 * math.pi * np.arange(T, dtype=np.float32) / T).astype(np.float32)
    f = np.linspace(1e-4, 15, 16, dtype=np.float32)
    fw = (w[:, None] * f[None, :]).astype(np.float32)
    z = np.concatenate([t[:, None], np.cos(fw), -np.sin(fw)], axis=-1).astype(np.float32)
    pos = np.zeros(N, dtype=np.int64)
    pos[:T] = np.arange(T)
    pos[T + 1:] = N - np.arange(T + 1, N)
    mask = np.ones(N, dtype=np.float32)
    mask[T] = 0.0
    zT = np.ascontiguousarray(z[pos].T)
    tpos = t[pos]
    NS = N // 128
    tposT = np.ascontiguousarray((-tpos).reshape(NS, 128).T)
    maskT = np.ascontiguousarray(mask.reshape(NS, 128).T)
    TT = T // 128
    NFT = TT + 1
    NFP = NFT * 128
    k = np.arange(NFP, dtype=np.float64)
    tt = np.arange(T, dtype=np.float64)
    ang = 2.0 * math.pi * np.outer(tt, k) / N
    valid = (k <= T).astype(np.float64)
    Fc = np.cos(ang) * valid
    Fs = -np.sin(ang) * valid
    FW = np.stack([Fc, Fs], 0).reshape(2, TT, 128, NFT, 128).transpose(3, 0, 2, 1, 4)
    FW = _bf(FW.reshape(NFT, 2, 128, TT * 128))
    ck = np.where((k == 0) | (k == T), 1.0, 2.0) * valid / N
    Ic = (np.cos(ang) * ck).T
    Is = (-np.sin(ang) * ck).T
    IV = np.stack([Ic, Is], 0).reshape(2, NFT, 128, TT, 128).transpose(3, 0, 2, 1, 4)
    IV = _bf(IV.reshape(TT, 2, 128, NFT * 128))
    return dict(zT=zT, tposT=tposT, maskT=maskT, FW=FW, IV=IV)


def vecT(v, width=128):
    n = v.shape[-1]
    return np.ascontiguousarray(v.reshape(v.shape[:-1] + (n // width, width)).swapaxes(-1, -2))


def build_program(T, TC, L, NS=2, dbg=()):
    nc = bass.Bass("TRN2", target_bir_lowering=False)
    S = Sched(nc)
    KT = D // 128

    def din(name, shape, dt=F32):
        return nc.dram_tensor(name, list(shape), dt, kind="ExternalInput").ap()

    def dscr(name, shape, dt=F32):
        return nc.dram_tensor(name, list(shape), dt, kind=("ExternalOutput" if dbg else "Internal")).ap()

    x_in = din("x", [NS, T, D])
    ctx_in = din("ctx", [NS, TC, D])
    c3T = din("c3T", [128, KT, 3])
    mod_w = din("mod_w", [L, D, 6 * D])
    mod_b = din("mod_b", [L, 6 * D])
    n1gT = din("n1gT", [L, 128, KT])
    n2gT = din("n2gT", [L, 128, KT])
    n2g = din("n2g", [L, D])
    w_in = din("w_in", [L, D, IN_COLS])
    w_krs = din("w_krs", [L, D, 96])
    qagT = din("qagT", [L, 128, 2])
    wq = din("wq", [L, 256, NH * 96])
    wqs = din("wqs", [L, 256, NH * 96])
    kvagT = din("kvagT", [L, 128, 1])
    wk = din("wk", [L, 128, NH * 64])
    wv = din("wv", [L, 128, NH * 64])
    dwwT = din("dwwT", [L, 128, 2, CONV_K])
    dwbT = din("dwbT", [L, 128, 2])
    clngT = din("clngT", [L, 128, 2])
    clnbT = din("clnbT", [L, 128, 2])
    hsw = din("hsw", [L, 3, 768])
    hsb = din("hsb", [L, 768])
    hw1 = din("hw1", [L, 33, 64])
    hb1T = din("hb1T", [L, 64, 1])
    hw2 = din("hw2", [L, 64, 64])
    hb2T = din("hb2T", [L, 64, 1])
    hw3 = din("hw3", [L, 64, 1024])
    hfrT = din("hfrT", [L, 64, 2])
    hdec = din("hdec", [L, 256])
    hskip = din("hskip", [L, 2, 256])
    gnT = din("gnT", [L, 128, 8])
    gnaT = din("gnaT", [L, 64, 8])
    gn = din("gn", [L, D])
    w_out = din("w_out", [L, D, D])
    router = din("router", [L, D, N_EXP])
    w_gate = din("w_gate", [L, N_EXP, D, D])
    w_up = din("w_up", [L, N_EXP, D, D])
    w_down = din("w_down", [L, N_EXP, D, D])
    fng = din("fng", [D])
    ident_f = din("ident_f", [128, 128])
    ident_b = din("ident_b", [128, 128], BF16)
    triu_in = din("triu", [128, 128])
    ropeC = din("ropeC", [32, T])
    ropeS = din("ropeS", [32, T])
    sgn_in = din("sgn", [128, 1])
    hyc = {}
    for (nm, TTT) in (("L", T), ("C", TC)):
        NSL = 2 * TTT // 128
        TTn = TTT // 128
        NFT = TTn + 1
        hyc[nm] = dict(
            zT=din("hy_zT_" + nm, [33, 2 * TTT]),
            tposT=din("hy_tposT_" + nm, [128, NSL]),
            maskT=din("hy_maskT_" + nm, [128, NSL]),
            FW=din("hy_FW_" + nm, [NFT, 2, 128, TTn * 128], BF16),
            IV=din("hy_IV_" + nm, [TTn, 2, 128, NFT * 128], BF16),
            T=TTT, TT=TTn, NFT=NFT, NSL=NSL)
    out = nc.dram_tensor("out", [NS, T, D], F32, kind="ExternalOutput").ap()

    NKEY = T + TC
    MOD = dscr("MOD", [L, 3, 6 * D])
    seqs = []
    for s in range(NS):
        seqs.append(dict(kind="L", s=s, j=s, T=T, rope=True, kbase=0, nkeys=NKEY, x0=x_in[s]))
    for s in range(NS):
        seqs.append(dict(kind="C", s=s, j=2, T=TC, rope=False, kbase=T, nkeys=TC, x0=ctx_in[s]))
    for q in seqs:
        nm = "%s%d" % (q["kind"], q["s"])
        q["name"] = nm
        q["XA"] = dscr("XA_" + nm, [q["T"], D])
        q["XB"] = dscr("XB_" + nm, [q["T"], D])
        q["QT"] = dscr("QT_" + nm, [NH, 96, q["T"]], BF16)
        q["ATT"] = dscr("ATT_" + nm, [NH, 64, q["T"]])
        q["Z"] = dscr("Z_" + nm, [q["T"], 768], BF16)
        q["MIXT"] = dscr("MIXT_" + nm, [512, q["T"]], BF16)
        q["ACC"] = dscr("ACC_" + nm, [q["T"], D])
        q["H2"] = dscr("H2_" + nm, [q["T"], D], BF16)
        q["GATE"] = dscr("GATE_" + nm, [q["T"], N_EXP])
        q["IDX"] = dscr("IDX_" + nm, [q["T"], N_EXP], mybir.dt.int32)
        capq = 2 * q["T"] // N_EXP
        q["XG"] = [dscr("XG_%s_%d" % (nm, e_), [max(capq, 128), D], BF16) for e_ in range(N_EXP)]
        q["Y"] = [dscr("Y_%s_%d" % (nm, e_), [max(capq, 128), D]) for e_ in range(N_EXP)]
        q["RXG"] = [Res() for _ in range(N_EXP)]
        q["RY"] = [Res() for _ in range(N_EXP)]
        q["R"] = {k: Res(nm + k) for k in ("XA", "XB", "QT", "ATT", "Z", "MIXT", "ACC", "H2", "GATE", "IDX")}
        q["RACC"] = [Res() for _ in range(q["T"] // 128)]
    KTd = [dscr("KT_%d" % s, [NH, 96, NKEY], BF16) for s in range(NS)]
    Vd = [dscr("V_%d" % s, [NKEY, NH * 64], BF16) for s in range(NS)]
    RKT = [Res("KT%d" % s) for s in range(NS)]
    RV = [Res("V%d" % s) for s in range(NS)]
    RMOD = Res("MOD")
    GSPEC = {nm: dscr("GSPEC_" + nm, [hyc[nm]["NFT"] * 128, 2, 2, 512], BF16) for nm in ("L", "C")}
    ULO = {nm: dscr("ULO_" + nm, [hyc[nm]["NFT"] * 128, 2, 512]) for nm in ("L", "C")}
    RGSPEC = {nm: Res("GSPEC" + nm) for nm in ("L", "C")}
    RULO = {nm: Res("ULO" + nm) for nm in ("L", "C")}
    RCONST = Res("const")
    dbg_out = {}

    def dbg_tensor(name, shape, dt=F32):
        t = nc.dram_tensor("dbg_" + name, list(shape), dt, kind="ExternalOutput").ap()
        dbg_out[name] = t
        return t

    uid = [0]

    def sb(st, name, shape, dt=F32):
        uid[0] += 1
        return st.enter_context(nc.sbuf_tensor("%s_%d" % (name, uid[0]), list(shape), dt))

    def ps(st, name, shape=(128, 512), dt=F32):
        uid[0] += 1
        return st.enter_context(nc.psum_tensor("%s_%d" % (name, uid[0]), list(shape), dt))

    def load(q, out_ap, in_ap, R=(), W=(), **kw):
        S.dma(q, lambda e: e.dma_start(out=out_ap, in_=in_ap, **kw), reads=list(R) + [RCONST], writes=W)

    def store(q, out_ap, in_ap, R=(), W=()):
        S.dma(q, lambda e: e.dma_start(out=out_ap, in_=in_ap), reads=R, writes=W)

    def mm(out_ap, lhsT, rhs, start, stop, R, W):
        S.op("pe", lambda e: e.matmul(out_ap, lhsT=lhsT, rhs=rhs, start=start, stop=stop), reads=R, writes=W)

    def tr(out_ap, in_ap, ident, R, W):
        S.op("pe", lambda e: e.transpose(out=out_ap, in_=in_ap, identity=ident), reads=R, writes=W)

    def act(out_ap, in_ap, func, R, W, **kw):
        S.op("act", lambda e: e.activation(out=out_ap, in_=in_ap, func=func, **kw), reads=R, writes=W)

    def tt(eng, out_ap, a, b, op, R, W):
        S.op(eng, lambda e: e.tensor_tensor(out=out_ap, in0=a, in1=b, op=op), reads=R, writes=W)

    def ts(eng, out_ap, a, s1, s2, op0, op1, R, W, **kw):
        if s2 is None:
            S.op(eng, lambda e: e.tensor_scalar(out=out_ap, in0=a, scalar1=s1, scalar2=None, op0=op0, **kw), reads=R, writes=W)
        else:
            S.op(eng, lambda e: e.tensor_scalar(out=out_ap, in0=a, scalar1=s1, scalar2=s2, op0=op0, op1=op1, **kw), reads=R, writes=W)

    def stt(eng, out_ap, a, sc, b, op0, op1, R, W):
        S.op(eng, lambda e: e.scalar_tensor_tensor(out=out_ap, in0=a, scalar=sc, in1=b, op0=op0, op1=op1), reads=R, writes=W)

    def cp(eng, out_ap, in_ap, R, W):
        if eng == "act":
            S.op("act", lambda e: e.copy(out=out_ap, in_=in_ap), reads=R, writes=W)
        else:
            S.op(eng, lambda e: e.tensor_copy(out=out_ap, in_=in_ap), reads=R, writes=W)

    def memset(eng, ap, val, W):
        S.op(eng, lambda e: e.memset(ap, val), writes=W)

    def recip(out_ap, in_ap, R, W):
        S.op("dve", lambda e: e.reciprocal(out=out_ap, in_=in_ap), reads=R, writes=W)

    def rmax(out_ap, in_ap, R, W):
        S.op("dve", lambda e: e.reduce_max(out=out_ap, in_=in_ap, axis=AX.X), reads=R, writes=W)

    def rsqrt_to(dst, src, scale, Rs, Rd, npart=128):
        act(dst, src, AF.Sqrt, list(Rs) + [RC], [Rd], bias=eps_t[0:npart, 0:1], scale=scale)
        recip(dst, dst, [Rd], [Rd])

    def rsqrt_ip(eng, ap, scale, R, npart=128):
        rsqrt_to(ap, ap, scale, [R], R, npart)

    with ExitStack() as top:
        idf = sb(top, "idf", [128, 128]); idb = sb(top, "idb", [128, 128], BF16)
        ones_f = sb(top, "ones_f", [128, 128]); sgn = sb(top, "sgn_sb", [128, 1])
        RC = Res("consts_sb")
        load("sp", idf[:], ident_f[:, :], W=[RC])
        load("sp", idb[:], ident_b[:, :], W=[RC])
        triu = sb(top, "triu_sb", [128, 128])
        load("sp", triu[:], triu_in[:, :], W=[RC])
        load("sp", sgn[:], sgn_in[:, :], W=[RC])
        memset("dve", ones_f[:], 1.0, [RC])
        eps_t = sb(top, "eps_t", [128, 1])
        memset("dve", eps_t[:], EPS, [RC])

        with ExitStack() as st:
            c3 = sb(st, "c3", [128, KT, 3]); sc3 = sb(st, "sc3", [128, KT, 3])
            Rc3 = Res()
            load("sp", c3[:], c3T[:, :, :], W=[Rc3])
            act(sc3[:], c3[:], AF.Silu, [Rc3], [Rc3])
            wbuf = [sb(st, "modw%d" % i, [128, KT, 512]) for i in range(2)]
            Rw = [Res(), Res()]
            bb = [sb(st, "modb%d" % i, [3, 512]) for i in range(2)]
            Rb = [Res(), Res()]
            pm = [ps(st, "pmod%d" % i) for i in range(2)]
            Rp = [Res(), Res()]
            it = 0
            for l in range(L):
                for cb in range(12):
                    i = it % 2
                    it += 1
                    load("sp", wbuf[i][:], mod_w[l, :, cb * 512:(cb + 1) * 512].rearrange("(k p) n -> p k n", p=128), W=[Rw[i]])
                    load("act", bb[i][:], mod_b[l:l + 1, cb * 512:(cb + 1) * 512].broadcast_to([3, 512]), W=[Rb[i]])
                    for k in range(KT):
                        mm(pm[i][0:3, :], sc3[:, k, :], wbuf[i][:, k, :], k == 0, k == KT - 1, [Rc3, Rw[i]], [Rp[i]])
                    tt("dve", bb[i][:], pm[i][0:3, :], bb[i][:], ALU.add, [Rp[i], Rb[i]], [Rb[i]])
                    store("act", MOD[l, :, cb * 512:(cb + 1) * 512], bb[i][:], R=[Rb[i]], W=[RMOD])
        S.barrier()

        for l in range(L):
            last = (l == L - 1)
            act_seqs = [q for q in seqs if not (last and q["kind"] == "C")]
            for q in seqs:
                q["Xin"] = q["x0"] if l == 0 else q["XA"]
                q["RXin"] = RCONST if l == 0 else q["R"]["XA"]

            for q in seqs:
                kv_only = last and q["kind"] == "C"
                Tq = q["T"]
                CT = min(512, Tq)
                NCH = Tq // CT
                NTL = CT // 128
                j = q["j"]
                with ExitStack() as st0, ExitStack() as st:
                    RW = Res("p1w")
                    dww = sb(st0, "dww", [128, 2, CONV_K]); dwb = sb(st0, "dwb", [128, 2])
                    clg = sb(st0, "clg", [128, 2]); clb = sb(st0, "clb", [128, 2]); gnc = sb(st0, "gnc", [128, 8])
                    yglu = sb(st0, "yglu", [128, 2, Tq + 30], BF16); Ryg = Res("yglu")
                    win = sb(st, "win", [128, KT, IN_COLS], BF16)
                    for k in range(KT):
                        load("pool", win[:, k, :], w_in[l, k * 128:(k + 1) * 128, :], W=[RW])
                    wkrs = sb(st, "wkrs", [128, KT, 96], BF16)
                    load("pool", wkrs[:], w_krs[l].rearrange("(k p) n -> p k n", p=128), W=[RW])
                    wq_sb = sb(st, "wq_sb", [128, 2, NH * 96], BF16)
                    load("pool", wq_sb[:], wq[l].rearrange("(k p) n -> p k n", p=128), W=[RW])
                    wqs_sb = sb(st, "wqs_sb", [128, 2, NH * 96], BF16)
                    load("pool", wqs_sb[:], wqs[l].rearrange("(k p) n -> p k n", p=128), W=[RW])
                    wk_sb = sb(st, "wk_sb", [128, NH * 64], BF16)
                    load("pool", wk_sb[:], wk[l], W=[RW])
                    wv_sb = sb(st, "wv_sb", [128, NH * 64], BF16)
                    load("pool", wv_sb[:], wv[l], W=[RW])
                    whj = sb(st, "whj", [128, 3, KT, 768], BF16)
                    hsw_rep = sb(st, "hsw_rep", [128, 3, 768])
                    hsb_rep = sb(st, "hsb_rep", [128, 768])
                    if not kv_only:
                        load("sp", hsw_rep[:], hsw[l:l + 1].broadcast_to([128, 3, 768]), W=[RW])
                        load("sp", hsb_rep[:], hsb[l:l + 1, :].broadcast_to([128, 768]), W=[RW])
                        for jj in range(3):
                            for k in range(KT):
                                tt("pool" if k % 2 else "dve", whj[:, jj, k, :], win[:, k, OFF_HY:OFF_HY + 768], hsw_rep[:, jj, :], ALU.mult, [RW], [RW])
                    A1 = sb(st, "A1", [128, KT]); B1 = sb(st, "B1", [128, KT]); tmpv = sb(st, "tmpv", [128, KT])
                    RS = Res("p1s")
                    load("sp", tmpv[:], MOD[l, j, D:2 * D].rearrange("(k p) -> p k", p=128), R=[RMOD], W=[RS], allow_slow_non_contiguous=True)
                    load("sp", B1[:], MOD[l, j, 0:D].rearrange("(k p) -> p k", p=128), R=[RMOD], W=[RS], allow_slow_non_contiguous=True)
                    load("sp", A1[:], n1gT[l], W=[RS])
                    stt("dve", A1[:], tmpv[:], 1.0, A1[:], ALU.add, ALU.mult, [RS], [RS])
                    qag = sb(st, "qag", [128, 2]); kvag = sb(st, "kvag", [128, 1])
                    load("sp", qag[:], qagT[l], W=[RS]); load("sp", kvag[:], kvagT[l], W=[RS])
                    load("sp", dww[:], dwwT[l], W=[RS]); load("sp", dwb[:], dwbT[l], W=[RS])
                    load("sp", clg[:], clngT[l], W=[RS]); load("sp", clb[:], clnbT[l], W=[RS])
                    load("sp", gnc[:], gnT[l], W=[RS])
                    hT = [sb(st, "hT%d" % i, [128, KT, CT + 2], BF16) for i in range(3)]
                    RhT = [Res("hT%d" % i) for i in range(3)]
                    xt = [sb(st, "xt%d" % i, [128, D]) for i in range(NTL)]
                    Rxt = [Res() for _ in range(NTL)]
                    ssq = sb(st, "ssq", [128, 4]); Rssq = Res()
                    junk = sb(st, "junk", [128, D]); Rjunk = Res()
                    PB = [ps(st, "pb%d" % i) for i in range(8)]
                    RPB = [Res("pb%d" % i) for i in range(8)]
                    pbi = [0]

                    def nextp():
                        i = pbi[0] % 8
                        pbi[0] += 1
                        return PB[i], RPB[i]
                    uq = sb(st, "uq", [128, 2, CT]); Ruq = Res()
                    sq = sb(st, "sq", [128, 2, CT]); Rsq = Res()
                    rstd = sb(st, "rstd", [128, CT]); Rrstd = Res()
                    qn = sb(st, "qn", [128, 2, CT], BF16); Rqn = Res()
                    ukv = sb(st, "ukv", [128, CT]); Rukv = Res()
                    kvn = sb(st, "kvn", [128, CT], BF16); Rkvn = Res()
                    rc = sb(st, "rc", [96, CT]); rs_ = sb(st, "rs", [96, CT]); Rrope = Res()
                    t32a = sb(st, "t32a", [96, CT]); t32b = sb(st, "t32b", [96, CT]); Rt32 = Res()
                    qh = [sb(st, "qh%d" % i, [96, CT], BF16) for i in range(2)]; Rqh = [Res(), Res()]
                    kh = [sb(st, "kh%d" % i, [96, CT], BF16) for i in range(2)]; Rkh = [Res(), Res()]
                    kr = sb(st, "kr", [96, CT], BF16); Rkr = Res()
                    vt = [sb(st, "vt%d" % i, [128, NH * 64], BF16) for i in range(2)]; Rvt = [Res(), Res()]
                    sig = sb(st, "sig", [128, CT]); Rsig = Res()
                    zt = [sb(st, "zt%d" % i, [128, 768], BF16) for i in range(2)]; Rzt = [Res(), Res()]
                    if not kv_only:
                        memset("dve", yglu[:], 0.0, [Ryg])
                    for i in range(3):
                        memset("pool", hT[i][:], 0.0, [RhT[i]])

                    def hy_proj(c):
                        b = c % 3
                        for tl in range(NTL):
                            zi = (c * NTL + tl) % 2
                            for half in range(2):
                                p, Rp = nextp()
                                n = 0
                                for jj in range(3):
                                    for k in range(KT):
                                        mm(p[:, 0:384], hT[b][:, k, tl * 128 + jj: tl * 128 + jj + 128],
                                           whj[:, jj, k, half * 384:(half + 1) * 384], n == 0, n == 3 * KT - 1,
                                           [RhT[b], RW], [Rp])
                                        n += 1
                                tt("dve", zt[zi][:, half * 384:(half + 1) * 384], p[:, 0:384],
                                   hsb_rep[:, half * 384:(half + 1) * 384], ALU.add, [Rp, RW], [Rzt[zi]])
                            tok0 = c * CT + tl * 128
                            store("act", q["Z"][tok0:tok0 + 128, :], zt[zi][:], R=[Rzt[zi]], W=[q["R"]["Z"]])

                    for c in range(NCH):
                        b = c % 3
                        for tl in range(NTL):
                            tok0 = c * CT + tl * 128
                            load("sp", xt[tl][:], q["Xin"][tok0:tok0 + 128, :], R=[q["RXin"]], W=[Rxt[tl]])
                            act(junk[:], xt[tl][:], AF.Square, [Rxt[tl]], [Rjunk, Rssq], accum_out=ssq[:, tl:tl + 1])
                        rsqrt_ip("dve", ssq[:, 0:NTL], 1.0 / D, Rssq)
                        for tl in range(NTL):
                            ts("dve", xt[tl][:], xt[tl][:], ssq[:, tl:tl + 1], None, ALU.mult, None, [Rxt[tl], Rssq], [Rxt[tl]])
                        for k in range(KT):
                            p, Rp = nextp()
                            for tl in range(NTL):
                                tr(p[:, tl * 128:(tl + 1) * 128], xt[tl][:, k * 128:(k + 1) * 128], idf[:], [Rxt[tl], RC], [Rp])
                            ts("dve", hT[b][:, k, 1:CT + 1], p[:, 0:CT], A1[:, k:k + 1], B1[:, k:k + 1], ALU.mult, ALU.add,
                               [Rp, RS], [RhT[b]])
                        if c > 0:
                            pb_ = (c - 1) % 3
                            cp("pool", hT[b][:, :, 0:1], hT[pb_][:, :, CT:CT + 1], [RhT[pb_]], [RhT[b]])
                            cp("pool", hT[pb_][:, :, CT + 1:CT + 2], hT[b][:, :, 1:2], [RhT[b]], [RhT[pb_]])
                        else:
                            memset("pool", hT[b][:, :, 0:1], 0.0, [RhT[b]])
                        if c == NCH - 1:
                            memset("pool", hT[b][:, :, CT + 1:CT + 2], 0.0, [RhT[b]])
                        hc = lambda k: hT[b][:, k, 1:CT + 1]
                        c0 = c * CT
                        if q["rope"]:
                            load("sp", rc[64:96, :], ropeC[:, c0:c0 + CT], W=[Rrope])
                            load("sp", rs_[64:96, :], ropeS[:, c0:c0 + CT], W=[Rrope])

                        def rope_combine(dst, pa, Rpa, pb2, Rpb2, Rdst):
                            if q["rope"]:
                                tt("dve", t32a[64:96, :], pa, rc[64:96, :], ALU.mult, [Rpa, Rrope], [Rt32])
                                tt("dve", t32b[64:96, :], pb2, rs_[64:96, :], ALU.mult, [Rpb2, Rrope], [Rt32])
                                tt("dve", dst, t32a[64:96, :], t32b[64:96, :], ALU.add, [Rt32], [Rdst])
                            else:
                                cp("dve", dst, pa, [Rpa], [Rdst])

                        if not kv_only:
                            for cc in range(2):
                                p, Rp = nextp()
                                for k in range(KT):
                                    mm(p[:, 0:CT], win[:, k, cc * 128:(cc + 1) * 128], hc(k), k == 0, k == KT - 1, [RW, RhT[b]], [Rp])
                                cp("act", uq[:, cc, :], p[:, 0:CT], [Rp], [Ruq])
                                act(sq[:, cc, :], p[:, 0:CT], AF.Square, [Rp], [Rsq])
                            p, Rp = nextp()
                            for cc in range(2):
                                mm(p[:, 0:CT], ones_f[:], sq[:, cc, :], cc == 0, cc == 1, [RC, Rsq], [Rp])
                            cp("dve", rstd[:], p[:, 0:CT], [Rp], [Rrstd])
                            rsqrt_ip("dve", rstd[:], 1.0 / 256, Rrstd)
                            for cc in range(2):
                                stt("dve", qn[:, cc, :], uq[:, cc, :], qag[:, cc:cc + 1], rstd[:], ALU.mult, ALU.mult, [Ruq, RS, Rrstd], [Rqn])
                            for h in range(NH):
                                hi = h % 2
                                p, Rp = nextp()
                                for cc in range(2):
                                    mm(p[0:96, 0:CT], wq_sb[:, cc, h * 96:(h + 1) * 96], qn[:, cc, :], cc == 0, cc == 1, [RW, Rqn], [Rp])
                                p2, Rp2 = nextp()
                                if q["rope"]:
                                    for cc in range(2):
                                        mm(p2[0:96, 0:CT], wqs_sb[:, cc, h * 96:(h + 1) * 96], qn[:, cc, :], cc == 0, cc == 1, [RW, Rqn], [Rp2])
                                cp("act", qh[hi][0:64, :], p[0:64, 0:CT], [Rp], [Rqh[hi]])
                                rope_combine(qh[hi][64:96, :], p[64:96, 0:CT], Rp, p2[64:96, 0:CT], Rp2, Rqh[hi])
                                store("act", q["QT"][h, :, c0:c0 + CT], qh[hi][:], R=[Rqh[hi]], W=[q["R"]["QT"]])
                        p, Rp = nextp()
                        for k in range(KT):
                            mm(p[:, 0:CT], win[:, k, OFF_KV:OFF_KV + 128], hc(k), k == 0, k == KT - 1, [RW, RhT[b]], [Rp])
                        cp("act", ukv[:], p[:, 0:CT], [Rp], [Rukv])
                        act(sq[:, 0, :], p[:, 0:CT], AF.Square, [Rp], [Rsq])
                        p, Rp = nextp()
                        mm(p[:, 0:CT], ones_f[:], sq[:, 0, :], True, True, [RC, Rsq], [Rp])
                        cp("dve", rstd[:], p[:, 0:CT], [Rp], [Rrstd])
                        rsqrt_ip("dve", rstd[:], 1.0 / 128, Rrstd)
                        stt("dve", kvn[:], ukv[:], kvag[:, 0:1], rstd[:], ALU.mult, ALU.mult, [Rukv, RS, Rrstd], [Rkvn])
                        p, Rp = nextp()
                        for k in range(KT):
                            mm(p[0:96, 0:CT], win[:, k, OFF_KR - 64:OFF_KR + 32], hc(k), k == 0, k == KT - 1, [RW, RhT[b]], [Rp])
                        p2, Rp2 = nextp()
                        if q["rope"]:
                            for k in range(KT):
                                mm(p2[0:96, 0:CT], wkrs[:, k, :], hc(k), k == 0, k == KT - 1, [RW, RhT[b]], [Rp2])
                        rope_combine(kr[64:96, :], p[64:96, 0:CT], Rp, p2[64:96, 0:CT], Rp2, Rkr)
                        kb = q["kbase"] + c0
                        s_ = q["s"]
                        for h in range(NH):
                            hi = h % 2
                            p, Rp = nextp()
                            mm(p[0:64, 0:CT], wk_sb[:, h * 64:(h + 1) * 64], kvn[:], True, True, [RW, Rkvn], [Rp])
                            cp("act", kh[hi][0:64, :], p[0:64, 0:CT], [Rp], [Rkh[hi]])
                            cp("pool", kh[hi][64:96, :], kr[64:96, :], [Rkr], [Rkh[hi]])
                            store("act", KTd[s_][h, :, kb:kb + CT], kh[hi][:], R=[Rkh[hi]], W=[RKT[s_]])
                        for tl in range(NTL):
                            vi = tl % 2
                            p, Rp = nextp()
                            mm(p[:, 0:512], kvn[:, tl * 128:(tl + 1) * 128], wv_sb[:], True, True, [Rkvn, RW], [Rp])
                            cp("act", vt[vi][:], p[:, 0:512], [Rp], [Rvt[vi]])
                            store("act", Vd[s_][kb + tl * 128: kb + tl * 128 + 128, :], vt[vi][:], R=[Rvt[vi]], W=[RV[s_]])
                        if not kv_only:
                            for cc in range(2):
                                pa, Rpa = nextp()
                                for k in range(KT):
                                    mm(pa[:, 0:CT], win[:, k, OFF_CONV + cc * 128: OFF_CONV + (cc + 1) * 128], hc(k), k == 0, k == KT - 1, [RW, RhT[b]], [Rpa])
                                pg, Rpg = nextp()
                                for k in range(KT):
                                    mm(pg[:, 0:CT], win[:, k, OFF_CONV + 256 + cc * 128: OFF_CONV + 256 + (cc + 1) * 128], hc(k), k == 0, k == KT - 1, [RW, RhT[b]], [Rpg])
                                act(sig[:], pg[:, 0:CT], AF.Sigmoid, [Rpg], [Rsig])
                                tt("dve", yglu[:, cc, 15 + c0: 15 + c0 + CT], pa[:, 0:CT], sig[:], ALU.mult, [Rpa, Rsig], [Ryg])
                            if c > 0:
                                hy_proj(c - 1)
                    if not kv_only:
                        hy_proj(NCH - 1)

                    st.close()
                    S.barrier()
                    if not kv_only:
                        st = st0
                        PB = [ps(st, "pc%d" % i) for i in range(6)]
                        RPB = [Res("pc%d" % i) for i in range(6)]
                        pci = [0]

                        def nextp():
                            i = pci[0] % 6
                            pci[0] += 1
                            return PB[i], RPB[i]
                        dgw = sb(st, "dgw", [128, 2, CONV_K, 128], BF16); Rdgw = Res()
                        for cc in range(2):
                            for jj in range(CONV_K):
                                ts("dve" if (jj % 2) else "pool", dgw[:, cc, jj, :], idb[:], dww[:, cc, jj:jj + 1], None, ALU.mult, None, [RC, RS], [Rdgw])
                        accs = sb(st, "cacc", [128, 2, CT]); Racc = [Res(), Res()]
                        mean = sb(st, "cmean", [128, CT]); var = sb(st, "cvar", [128, CT]); Rst = Res()
                        sq2 = sb(st, "csq", [128, 2, CT]); Rsq2 = Res()
                        zc = sb(st, "czc", [128, 2, CT]); Rzc = Res()
                        cvo = [sb(st, "cvo%d" % i, [128, 2, CT], BF16) for i in range(2)]; Rcvo = [Res(), Res()]
                        for c in range(NCH):
                            c0 = c * CT
                            for cc in range(2):
                                pcv, Rpcv = nextp()
                                for jj in range(CONV_K):
                                    mm(pcv[:, 0:CT], dgw[:, cc, jj, :], yglu[:, cc, c0 + jj:c0 + jj + CT], jj == 0, jj == CONV_K - 1, [Rdgw, Ryg], [Rpcv])
                                ts("dve", accs[:, cc, :], pcv[:, 0:CT], dwb[:, cc:cc + 1], None, ALU.add, None, [Rpcv, RS], [Racc[cc]])
                            for cc in range(2):
                                act(sq2[:, cc, :], accs[:, cc, :], AF.Square, [Racc[cc]], [Rsq2])
                            p1, Rp1 = nextp()
                            for cc in range(2):
                                mm(p1[:, 0:CT], ones_f[:], accs[:, cc, :], cc == 0, cc == 1, [RC, Racc[cc]], [Rp1])
                            p2, Rp2 = nextp()
                            for cc in range(2):
                                mm(p2[:, 0:CT], ones_f[:], sq2[:, cc, :], cc == 0, cc == 1, [RC, Rsq2], [Rp2])
                            ts("dve", mean[:], p1[:, 0:CT], 1.0 / 256, None, ALU.mult, None, [Rp1], [Rst])
                            tt("dve", var[:], mean[:], mean[:], ALU.mult, [Rst], [Rst])
                            stt("dve", var[:], p2[:, 0:CT], 1.0 / 256, var[:], ALU.mult, ALU.subtract, [Rp2, Rst], [Rst])
                            rsqrt_to(var[:], var[:], 1.0, [Rst], Rst)
                            for cc in range(2):
                                tt("dve", zc[:, cc, :], accs[:, cc, :], mean[:], ALU.subtract, [Racc[cc], Rst], [Rzc])
                                tt("dve", zc[:, cc, :], zc[:, cc, :], var[:], ALU.mult, [Rzc, Rst], [Rzc])
                                act(zc[:, cc, :], zc[:, cc, :], AF.Silu, [Rzc, RS], [Rzc], bias=clb[:, cc:cc + 1], scale=clg[:, cc:cc + 1])
                                act(sq2[:, cc, :], zc[:, cc, :], AF.Square, [Rzc], [Rsq2])
                            p3, Rp3 = nextp()
                            for cc in range(2):
                                mm(p3[:, 0:CT], ones_f[:], sq2[:, cc, :], cc == 0, cc == 1, [RC, Rsq2], [Rp3])
                            rsqrt_to(var[:], p3[:, 0:CT], 1.0 / 256, [Rp3], Rst)
                            oi = c % 2
                            for cc in range(2):
                                stt("dve", cvo[oi][:, cc, :], zc[:, cc, :], gnc[:, 4 + cc:5 + cc], var[:], ALU.mult, ALU.mult, [Rzc, RS, Rst], [Rcvo[oi]])
                                store("act", q["MIXT"][cc * 128:(cc + 1) * 128, c0:c0 + CT], cvo[oi][:, cc, :], R=[Rcvo[oi]], W=[q["R"]["MIXT"]])
                S.barrier()

            scale = 96.0 ** -0.5
            for q in act_seqs:
                Tq = q["T"]
                CT = min(512, Tq)
                NCH = Tq // CT
                NK = q["nkeys"]
                NKT = NK // 128
                s_ = q["s"]
                kb0 = q["kbase"] if q["kind"] == "C" else 0
                with ExitStack() as st:
                    ktb = [sb(st, "ktb%d" % i, [96, NK], BF16) for i in range(2)]; Rktb = [Res(), Res()]
                    qtb = [sb(st, "qtb%d" % i, [96, Tq], BF16) for i in range(2)]; Rqtb = [Res(), Res()]
                    vab = [sb(st, "vab%d" % i, [128, NKT, 65], BF16) for i in range(2)]; Rvab = [Res(), Res()]
                    e65 = sb(st, "e65", [65, 64]); Re = Res()
                    memset("dve", e65[:], 0.0, [Re])
                    memset("dve", e65[64:65, :], 1.0, [Re])
                    for i in range(2):
                        memset("dve", vab[i][:, :, 64:65], 1.0, [Rvab[i]])
                    PS_ = [ps(st, "pss%d" % i) for i in range(4)]; RPS = [Res() for _ in range(4)]
                    PO = [ps(st, "pso%d" % i) for i in range(2)]; RPO = [Res(), Res()]
                    PR = ps(st, "psr"); RPR = Res()
                    pt = [sb(st, "pt%d" % i, [128, CT], BF16) for i in range(4)]; Rpt = [Res() for _ in range(4)]
                    osb = [sb(st, "osb%d" % i, [65, CT]) for i in range(2)]; Rosb = [Res(), Res()]
                    rcp = sb(st, "rcp", [64, CT]); Rrcp = Res()
                    ao = [sb(st, "ao%d" % i, [64, CT]) for i in range(2)]; Rao = [Res(), Res()]
                    it = 0
                    io = 0
                    for h in range(NH):
                        hb = h % 2
                        load("sp", ktb[hb][:], KTd[s_][h, :, kb0:kb0 + NK], R=[RKT[s_]], W=[Rktb[hb]])
                        load("sp", qtb[hb][:], q["QT"][h], R=[q["R"]["QT"]], W=[Rqtb[hb]])
                        load("sp", vab[hb][:, :, 0:64], Vd[s_][kb0:kb0 + NK, h * 64:(h + 1) * 64].rearrange("(kt p) d -> p kt d", p=128),
                             R=[RV[s_]], W=[Rvab[hb]])
                        for c in range(NCH):
                            c0 = c * CT
                            oi = io % 2
                            io += 1
                            LA = 2
                            for kk in range(NKT + LA):
                                if kk < NKT:
                                    kt = kk
                                    i4 = kt % 4
                                    mm(PS_[i4][:, 0:CT], ktb[hb][:, kt * 128:(kt + 1) * 128], qtb[hb][:, c0:c0 + CT], True, True,
                                       [Rktb[hb], Rqtb[hb]], [RPS[i4]])
                                    act(pt[i4][:], PS_[i4][:, 0:CT], AF.Exp, [RPS[i4]], [Rpt[i4]], scale=scale)
                                if kk >= LA:
                                    kt = kk - LA
                                    i4 = kt % 4
                                    mm(PO[oi][0:65, 0:CT], vab[hb][:, kt, :], pt[i4][:], kt == 0, kt == NKT - 1, [Rvab[hb], Rpt[i4]], [RPO[oi]])
                            cp("dve", osb[oi][:], PO[oi][0:65, 0:CT], [RPO[oi]], [Rosb[oi]])
                            mm(PR[0:64, 0:CT], e65[:], osb[oi][:], True, True, [Re, Rosb[oi]], [RPR])
                            recip(rcp[:], PR[0:64, 0:CT], [RPR], [Rrcp])
                            tt("dve", ao[oi][:], osb[oi][0:64, :], rcp[:], ALU.mult, [Rosb[oi], Rrcp], [Rao[oi]])
                            store("act", q["ATT"][h, :, c0:c0 + CT], ao[oi][:], R=[Rao[oi]], W=[q["R"]["ATT"]])
                S.barrier()

            for nm in (("L", "C") if not last else ("L",)):
                hc_ = hyc[nm]
                Th = hc_["T"]; TTn = hc_["TT"]; NFT = hc_["NFT"]; NSL = hc_["NSL"]
                sq_ = [q for q in act_seqs if q["kind"] == nm]
                with ExitStack() as st:
                    RH = Res("hyw")
                    w1 = sb(st, "hw1", [33, 64]); w2 = sb(st, "hw2", [64, 64]); w3 = sb(st, "hw3", [64, 1024])
                    b1 = sb(st, "hb1", [64, 1]); b2 = sb(st, "hb2", [64, 1]); fr = sb(st, "hfr", [64, 2])
                    load("sp", w1[:], hw1[l], W=[RH]); load("sp", w2[:], hw2[l], W=[RH]); load("sp", w3[:], hw3[l], W=[RH])
                    load("sp", b1[:], hb1T[l], W=[RH]); load("sp", b2[:], hb2T[l], W=[RH]); load("sp", fr[:], hfrT[l], W=[RH])
                    fb = sb(st, "hfb", [64, 2])
                    tt("dve", fb[:, 0:1], b1[:], fr[:, 0:1], ALU.mult, [RH], [RH])
                    tt("dve", fb[:, 1:2], b2[:], fr[:, 1:2], ALU.mult, [RH], [RH])
                    ts("dve", fb[:], fb[:], 1.0 / (2 * math.pi), 8.5, ALU.mult, ALU.add, [RH], [RH])
                    ts("dve", fr[:], fr[:], 1.0 / (2 * math.pi), None, ALU.mult, None, [RH], [RH])
                    adec = sb(st, "adec", [128, 256])
                    load("sp", adec[:], hdec[l:l + 1, :].broadcast_to([128, 256]), W=[RH])
                    stt("dve", adec[:], adec[:], -1.0, adec[:], ALU.mult, ALU.max, [RH], [RH])
                    tpos = sb(st, "tpos", [128, NSL]); maskc = sb(st, "maskc", [128, NSL])
                    load("sp", tpos[:], hc_["tposT"], W=[RH]); load("sp", maskc[:], hc_["maskT"], W=[RH])
                    skp = sb(st, "skp", [128, 2, 2, 256])
                    for s2 in range(2):
                        load("sp", skp[:, :, s2, :], hskip[l:l + 1].broadcast_to([128, 2, 256]), W=[RH])
                    gnr = sb(st, "gnr", [128, 256])
                    load("sp", gnr[:], gn[l:l + 1, 768:1024].broadcast_to([128, 256]), W=[RH])
                    data = sb(st, "hdata", [128, TTn, 512], BF16); Rdata = Res("hdata")
                    fwb = [sb(st, "fwb%d" % i, [128, 2, TTn * 128], BF16) for i in range(2)]; Rfwb = [Res(), Res()]
                    stf = ExitStack()
                    hid2 = sb(stf, "hid2", [64, 2 * Th]); Rhid2 = Res()
                    PH = [ps(st, "ph%d" % i) for i in range(6)]; RPH = [Res() for _ in range(6)]
                    PSS = ps(st, "phss"); RPSS = Res()
                    phi = [0]

                    def nph():
                        i = phi[0] % 6
                        phi[0] += 1
                        return PH[i], RPH[i]
                    zb = [sb(stf, "zb%d" % i, [33, 512]) for i in range(2)]; Rzb = [Res(), Res()]
                    h1 = sb(stf, "h1", [64, 512]); Rh1 = Res()
                    NCB = (2 * Th) // 512

                    h1i = sb(stf, "h1i", [64, 512], mybir.dt.int32); h1k = sb(stf, "h1k", [64, 512])

                    def sin_layer(dst, src_ps, Rsrc, li, Rdst):
                        ts("dve", h1[:], src_ps, fr[:, li:li + 1], fb[:, li:li + 1], ALU.mult, ALU.add, [Rsrc, RH], [Rh1])
                        cp("dve", h1i[:], h1[:], [Rh1], [Rh1])
                        cp("dve", h1k[:], h1i[:], [Rh1], [Rh1])
                        tt("dve", h1[:], h1[:], h1k[:], ALU.subtract, [Rh1], [Rh1])
                        ts("dve", h1k[:], h1[:], 0.5, None, ALU.is_gt, None, [Rh1], [Rh1])
                        tt("dve", h1[:], h1[:], h1k[:], ALU.subtract, [Rh1], [Rh1])
                        ts("dve", h1[:], h1[:], -0.5, 0.5, ALU.max, ALU.min, [Rh1], [Rh1])
                        act(dst, h1[:], AF.Sin, [Rh1], [Rdst], scale=-6.283185)
                    hA = sb(stf, "hA", [64, 512]); RhA = Res()
                    for cb in range(NCB):
                        i = cb % 2
                        load("sp", zb[i][:], hc_["zT"][:, cb * 512:(cb + 1) * 512], W=[Rzb[i]])
                        p, Rp = nph()
                        mm(p[0:64, :], w1[:], zb[i][:], True, True, [RH, Rzb[i]], [Rp])
                        sin_layer(hA[:], p[0:64, :], Rp, 0, RhA)
                        p, Rp = nph()
                        mm(p[0:64, :], w2[:], hA[:], True, True, [RH, RhA], [Rp])
                        sin_layer(hid2[:, cb * 512:(cb + 1) * 512], p[0:64, :], Rp, 1, Rhid2)
                    dec = sb(stf, "hdecay", [128, 256]); Rdec = Res()
                    gsq = sb(stf, "gsq", [128, 512]); Rgsq = Res()
                    graw = sb(stf, "graw", [128, 512]); Rgraw = Res()
                    w3v = w3[:].rearrange("p (o d c) -> p o d c", o=2, d=2)
                    utile = sb(stf, "utile", [128, 2, 512]); Rut = Res()
                    gtile = [sb(stf, "gtile%d" % i, [128, 2, 2, 512], BF16) for i in range(2)]; Rgt = [Res(), Res()]
                    rn = sb(stf, "rnorm", [128, 512]); Rrn = Res()
                    fwi = [0]

                    def forward(consume):
                        for ft in range(NFT):
                            i = fwi[0] % 2
                            fwi[0] += 1
                            load("sp", fwb[i][:], hc_["FW"][ft].rearrange("r p n -> p r n"), W=[Rfwb[i]])
                            pr, Rpr = nph()
                            pi_, Rpi = nph()
                            for tc in range(TTn):
                                mm(pr[:], fwb[i][:, 0, tc * 128:(tc + 1) * 128], data[:, tc, :], tc == 0, tc == TTn - 1, [Rfwb[i], Rdata], [Rpr])
                            for tc in range(TTn):
                                mm(pi_[:], fwb[i][:, 1, tc * 128:(tc + 1) * 128], data[:, tc, :], tc == 0, tc == TTn - 1, [Rfwb[i], Rdata], [Rpi])
                            consume(ft, pr, Rpr, pi_, Rpi)

                    nss = [0]
                    for half in range(2):
                        for m in range(TTn):
                            slot = half * TTn + m
                            p, Rp = nph()
                            for o in range(2):
                                mm(p[:, o * 256:(o + 1) * 256], hid2[:, slot * 128:(slot + 1) * 128], w3v[:, o, half, :], True, True, [Rhid2, RH], [Rp])
                            act(dec[:], adec[:], AF.Exp, [RH], [Rdec], scale=tpos[:, slot:slot + 1])
                            for o in range(2):
                                stt("dve", graw[:, o * 256:(o + 1) * 256], p[:, o * 256:(o + 1) * 256], maskc[:, slot:slot + 1], dec[:], ALU.mult, ALU.mult,
                                    [Rp, RH, Rdec], [Rgraw])
                            cp("pool", data[:, m, :], graw[:], [Rgraw], [Rdata])
                            act(gsq[:], graw[:], AF.Square, [Rgraw], [Rgsq])
                            mm(PSS[:], ones_f[:], gsq[:], nss[0] == 0, nss[0] == 2 * TTn - 1, [RC, Rgsq], [RPSS])
                            nss[0] += 1
                        if half == 0:
                            def consume_lo(ft, pr, Rpr, pi_, Rpi):
                                cp("dve", utile[:, 0, :], pr[:], [Rpr], [Rut])
                                cp("act", utile[:, 1, :], pi_[:], [Rpi], [Rut])
                                store("act", ULO[nm][ft * 128:(ft + 1) * 128], utile[:], R=[Rut], W=[RULO[nm]])
                            forward(consume_lo)
                        else:
                            rsqrt_to(rn[:], PSS[:], 1.0, [RPSS], Rrn)

                            def consume_hi(ft, pr, Rpr, pi_, Rpi):
                                gi = ft % 2
                                load("sp", utile[:], ULO[nm][ft * 128:(ft + 1) * 128], R=[RULO[nm]], W=[Rut])
                                for ri, (pp, Rpp) in enumerate(((pr, Rpr), (pi_, Rpi))):
                                    stt("dve", utile[:, ri, :], pp[:], sgn[:, 0:1], utile[:, ri, :], ALU.mult, ALU.add, [Rpp, RC, Rut], [Rut])
                                    for o in range(2):
                                        for s2 in range(2):
                                            tt("dve", gtile[gi][:, o, ri, s2 * 256:(s2 + 1) * 256], utile[:, ri, o * 256:(o + 1) * 256],
                                               rn[:, o * 256:(o + 1) * 256], ALU.mult, [Rut, Rrn], [Rgt[gi]])
                                store("act", GSPEC[nm][ft * 128:(ft + 1) * 128], gtile[gi][:], R=[Rgt[gi]], W=[RGSPEC[nm]])
                            forward(consume_hi)

                    stf.close()
                    S.barrier()
                    spec = sb(st, "hspec", [128, NFT, 2, 512], BF16); Rspec = Res("hspec")
                    ivb = [sb(st, "ivb%d" % i, [128, 2, NFT * 128], BF16) for i in range(2)]; Rivb = [Res(), Res()]
                    for si, q in enumerate(sq_):
                        load("sp", data[:, :, si * 256:(si + 1) * 256], q["Z"][:, 0:256].rearrange("(tt p) c -> p tt c", p=128),
                             R=[q["R"]["Z"]], W=[Rdata])
                    gl = [sb(st, "gl%d" % i, [128, 2, 512], BF16) for i in range(2)]; Rgl = [Res(), Res()]
                    ta = sb(st, "hta", [128, 512]); tb = sb(st, "htb", [128, 512]); Rtab = Res()
                    xg = [sb(st, "xg%d" % i, [128, 512], BF16) for i in range(2)]; Rxg = [Res(), Res()]
                    yt = sb(st, "hyt", [128, 512]); Ryt = Res()
                    hss = sb(st, "hss", [128, 2]); Rhss = Res()
                    hyo = sb(st, "hyo", [128, 512], BF16); Rhyo = Res()
                    hyT = [sb(st, "hyT%d" % i, [128, 2, 2, 128], BF16) for i in range(2)]; RhyT = [Res(), Res()]
                    PT = ps(st, "phT", [128, 512], BF16); RPT = Res()
                    ivi = [0]
                    for o in range(2):
                        def consume_sig(ft, pr, Rpr, pi_, Rpi, o=o):
                            gi = ft % 2
                            load("sp", gl[gi][:], GSPEC[nm][ft * 128:(ft + 1) * 128, o], R=[RGSPEC[nm]], W=[Rgl[gi]])
                            tt("dve", ta[:], pr[:], gl[gi][:, 0, :], ALU.mult, [Rpr, Rgl[gi]], [Rtab])
                            tt("dve", tb[:], pi_[:], gl[gi][:, 1, :], ALU.mult, [Rpi, Rgl[gi]], [Rtab])
                            tt("dve", spec[:, ft, 0, :], ta[:], tb[:], ALU.subtract, [Rtab], [Rspec])
                            tt("dve", ta[:], pr[:], gl[gi][:, 1, :], ALU.mult, [Rpr, Rgl[gi]], [Rtab])
                            tt("dve", tb[:], pi_[:], gl[gi][:, 0, :], ALU.mult, [Rpi, Rgl[gi]], [Rtab])
                            tt("dve", spec[:, ft, 1, :], ta[:], tb[:], ALU.add, [Rtab], [Rspec])
                        forward(consume_sig)
                        for tti in range(TTn):
                            i = ivi[0] % 2
                            ivi[0] += 1
                            load("sp", ivb[i][:], hc_["IV"][tti].rearrange("r p n -> p r n"), W=[Rivb[i]])
                            xi = tti % 2
                            for si, q in enumerate(sq_):
                                load("act", xg[xi][:, si * 256:(si + 1) * 256], q["Z"][tti * 128:(tti + 1) * 128, 256 * (o + 1):256 * (o + 2)],
                                     R=[q["R"]["Z"]], W=[Rxg[xi]])
                            py, Rpy = nph()
                            n = 0
                            for ri in range(2):
                                for fc in range(NFT):
                                    mm(py[:], ivb[i][:, ri, fc * 128:(fc + 1) * 128], spec[:, fc, ri, :], n == 0, n == 2 * NFT - 1, [Rivb[i], Rspec], [Rpy])
                                    n += 1
                            tt("dve", yt[:], data[:, tti, :], skp[:, o].rearrange("p s c -> p (s c)"), ALU.mult, [Rdata, RH], [Ryt])
                            tt("dve", yt[:], yt[:], py[:], ALU.add, [Ryt, Rpy], [Ryt])
                            if o == 0:
                                tt("dve", data[:, tti, :], yt[:], xg[xi][:], ALU.mult, [Ryt, Rxg[xi]], [Rdata])
                            else:
                                tt("dve", yt[:], yt[:], xg[xi][:], ALU.mult, [Ryt, Rxg[xi]], [Ryt])
                                for si in range(len(sq_)):
                                    act(ta[:, 0:256], yt[:, si * 256:(si + 1) * 256], AF.Square, [Ryt], [Rtab, Rhss], accum_out=hss[:, si:si + 1])
                                rsqrt_ip("dve", hss[:, 0:len(sq_)], 1.0 / 256, Rhss)
                                for si in range(len(sq_)):
                                    stt("dve", hyo[:, si * 256:(si + 1) * 256], yt[:, si * 256:(si + 1) * 256], hss[:, si:si + 1], gnr[:], ALU.mult, ALU.mult,
                                        [Ryt, Rhss, RH], [Rhyo])
                                ti = tti % 2
                                for si in range(len(sq_)):
                                    for cc in range(2):
                                        tr(PT[:, (si * 2 + cc) * 128:(si * 2 + cc + 1) * 128], hyo[:, si * 256 + cc * 128: si * 256 + (cc + 1) * 128], idb[:], [Rhyo, RC], [RPT])
                                cp("dve", hyT[ti][:].rearrange("p s c t -> p (s c t)"), PT[:, :], [RPT], [RhyT[ti]])
                                for si, q in enumerate(sq_):
                                    for cc in range(2):
                                        store("act", q["MIXT"][256 + cc * 128: 256 + (cc + 1) * 128, tti * 128:(tti + 1) * 128], hyT[ti][:, si, cc, :],
                                              R=[RhyT[ti]], W=[q["R"]["MIXT"]])
                S.barrier()

            for q in act_seqs:
                Tq = q["T"]
                CT = min(512, Tq)
                NCH = Tq // CT
                NTL = CT // 128
                NTT = Tq // 128
                j = q["j"]
                cap = 2 * Tq // N_EXP
                with ExitStack() as st:
                    RW = Res("p5w")
                    woa = sb(st, "woa", [64, NH, D], BF16)
                    load("pool", woa[:], w_out[l, 0:512, :].rearrange("(h p) n -> p h n", p=64), W=[RW])
                    wob = sb(st, "wob", [128, 4, D], BF16)
                    load("pool", wob[:], w_out[l, 512:1024, :].rearrange("(k p) n -> p k n", p=128), W=[RW])
                    gna = sb(st, "gna", [64, NH]); load("sp", gna[:], gnaT[l], W=[RW])
                    g1r = sb(st, "g1r", [128, D]); load("sp", g1r[:], MOD[l, j:j + 1, 2 * D:3 * D].broadcast_to([128, D]), R=[RMOD], W=[RW])
                    A2r = sb(st, "A2r", [128, D]); B2r = sb(st, "B2r", [128, D]); n2r = sb(st, "n2r", [128, D])
                    load("sp", A2r[:], MOD[l, j:j + 1, 4 * D:5 * D].broadcast_to([128, D]), R=[RMOD], W=[RW])
                    load("sp", B2r[:], MOD[l, j:j + 1, 3 * D:4 * D].broadcast_to([128, D]), R=[RMOD], W=[RW])
                    load("sp", n2r[:], n2g[l:l + 1, :].broadcast_to([128, D]), W=[RW])
                    stt("dve", A2r[:], A2r[:], 1.0, n2r[:], ALU.add, ALU.mult, [RW], [RW])
                    rw = sb(st, "rw", [128, KT, N_EXP])
                    load("sp", rw[:], router[l].rearrange("(k p) e -> p k e", p=128), W=[RW])
                    att = sb(st, "att", [64, NH, CT]); Ratt = Res()
                    asq = sb(st, "asq", [64, NH, CT]); Rasq = Res()
                    ones64 = ones_f[0:64, :]
                    rstd = sb(st, "arstd", [128, CT]); Rrstd = Res()
                    attn = sb(st, "attn", [64, NH, CT], BF16); Rattn = Res()
                    mixb = sb(st, "mixb", [128, 4, CT], BF16); Rmixb = Res()
                    xt = [sb(st, "x5t%d" % i, [128, D]) for i in range(2)]; Rxt = [Res(), Res()]
                    ot = sb(st, "o5t", [128, D]); Rot = Res()
                    junk = sb(st, "junk5", [128, D]); Rjunk = Res()
                    ss2 = sb(st, "ss2", [128, 1]); Rss2 = Res()
                    xn = sb(st, "xn5", [128, D]); Rxn = Res()
                    h2r = [sb(st, "h2r%d" % i, [128, D], BF16) for i in range(2)]; Rh2r = [Res(), Res()]
                    h2f = sb(st, "h2f", [128, D]); Rh2f = Res()
                    h2T = sb(st, "h2T", [128, KT, 128]); Rh2T = Res()
                    lg = sb(st, "lg", [128, N_EXP]); Rlg = Res()
                    mx = sb(st, "mx", [128, 1]); Rmx = Res()
                    aff = sb(st, "aff", [128, NTT, N_EXP]); Raff = Res("aff")
                    affT = sb(st, "affT", [N_EXP, Tq]); RaffT = Res()
                    PW = [ps(st, "pw%d" % i) for i in range(6)]; RPW = [Res() for _ in range(6)]
                    pwi = [0]

                    def npw():
                        i = pwi[0] % 6
                        pwi[0] += 1
                        return PW[i], RPW[i]
                    for c in range(NCH):
                        c0 = c * CT
                        for h in range(NH):
                            load("sp", att[:, h, :], q["ATT"][h, :, c0:c0 + CT], R=[q["R"]["ATT"]], W=[Ratt])
                        load("sp", mixb[:], q["MIXT"][:, c0:c0 + CT].rearrange("(k p) t -> p k t", p=128), R=[q["R"]["MIXT"]], W=[Rmixb])
                        act(asq[:], att[:], AF.Square, [Ratt], [Rasq])
                        p, Rp = npw()
                        for h in range(NH):
                            mm(p[:, 0:CT], ones64, asq[:, h, :], h == 0, h == NH - 1, [RC, Rasq], [Rp])
                        cp("dve", rstd[:], p[:, 0:CT], [Rp], [Rrstd])
                        rsqrt_ip("dve", rstd[:], 1.0 / 512, Rrstd)
                        for h in range(NH):
                            stt("dve", attn[:, h, :], att[:, h, :], gna[:, h:h + 1], rstd[0:64, :], ALU.mult, ALU.mult, [Ratt, RW, Rrstd], [Rattn])
                        for tl in range(NTL):
                            tok0 = c0 + tl * 128
                            tile_i = tok0 // 128
                            xi = tile_i % 2
                            load("sp", xt[xi][:], q["Xin"][tok0:tok0 + 128, :], R=[q["RXin"]], W=[Rxt[xi]])
                            for half in range(2):
                                p, Rp = npw()
                                n = 0
                                for h in range(NH):
                                    mm(p[:, :], attn[:, h, tl * 128:(tl + 1) * 128], woa[:, h, half * 512:(half + 1) * 512], n == 0, False, [Rattn, RW], [Rp])
                                    n += 1
                                for k in range(4):
                                    mm(p[:, :], mixb[:, k, tl * 128:(tl + 1) * 128], wob[:, k, half * 512:(half + 1) * 512], False, k == 3, [Rmixb, RW], [Rp])
                                tt("dve", ot[:, half * 512:(half + 1) * 512], p[:, :], g1r[:, half * 512:(half + 1) * 512], ALU.mult, [Rp, RW], [Rot])
                            tt("dve", xt[xi][:], xt[xi][:], ot[:], ALU.add, [Rxt[xi], Rot], [Rxt[xi]])
                            store("act", q["XB"][tok0:tok0 + 128, :], xt[xi][:], R=[Rxt[xi]], W=[q["R"]["XB"]])
                            act(junk[:], xt[xi][:], AF.Square, [Rxt[xi]], [Rjunk, Rss2], accum_out=ss2[:, 0:1])
                            rsqrt_ip("dve", ss2[:], 1.0 / D, Rss2)
                            stt("dve", h2f[:], xt[xi][:], ss2[:, 0:1], A2r[:], ALU.mult, ALU.mult, [Rxt[xi], Rss2, RW], [Rh2f])
                            tt("dve", h2f[:], h2f[:], B2r[:], ALU.add, [Rh2f, RW], [Rh2f])
                            hi = tile_i % 2
                            cp("pool", h2r[hi][:], h2f[:], [Rh2f], [Rh2r[hi]])
                            store("pool", q["H2"][tok0:tok0 + 128, :], h2r[hi][:], R=[Rh2r[hi]], W=[q["R"]["H2"]])
                            for k in range(KT):
                                if k % 4 == 0:
                                    p, Rp = npw()
                                tr(p[:, (k % 4) * 128:(k % 4 + 1) * 128], h2f[:, k * 128:(k + 1) * 128], idf[:], [Rh2f, RC], [Rp])
                                if k % 4 == 3:
                                    cp("act", h2T[:, k - 3:k + 1, :].rearrange("p k t -> p (k t)"), p[:, :], [Rp], [Rh2T])
                            p, Rp = npw()
                            for k in range(KT):
                                mm(p[:, 0:N_EXP], h2T[:, k, :], rw[:, k, :], k == 0, k == KT - 1, [Rh2T, RW], [Rp])
                            cp("dve", lg[:], p[:, 0:N_EXP], [Rp], [Rlg])
                            rmax(mx[:], lg[:], [Rlg], [Rmx])
                            ts("dve", mx[:], mx[:], -1.0, None, ALU.mult, None, [Rmx], [Rmx])
                            act(lg[:], lg[:], AF.Exp, [Rlg, Rmx], [Rlg, Rss2], bias=mx[:, 0:1], accum_out=ss2[:, 0:1])
                            recip(ss2[:], ss2[:], [Rss2], [Rss2])
                            ts("dve", aff[:, tile_i, :], lg[:], ss2[:, 0:1], None, ALU.mult, None, [Rlg, Rss2], [Raff])
                            p, Rp = npw()
                            tr(p[0:N_EXP, 0:128], aff[:, tile_i, :], idf[:], [Raff, RC], [Rp])
                            cp("act", affT[:, tok0:tok0 + 128], p[0:N_EXP, 0:128], [Rp], [RaffT])
                    lo = sb(st, "lo", [N_EXP, 1]); mid = sb(st, "mid", [N_EXP, 1]); cnt = sb(st, "cnt", [N_EXP, 1]); Rb = Res()
                    cmpj = sb(st, "cmpj", [N_EXP, Tq]); Rcmp = Res()
                    memset("dve", lo[:], 0.0, [Rb])
                    for it_ in range(1, 29):
                        w_ = 2.0 ** (-it_)
                        ts("dve", mid[:], lo[:], w_, None, ALU.add, None, [Rb], [Rb])
                        ts("dve", cmpj[:], affT[:], mid[:, 0:1], 0.0, ALU.is_ge, ALU.add, [RaffT, Rb], [Rcmp, Rb], accum_out=cnt[:, 0:1])
                        ts("dve", cnt[:], cnt[:], cap - 0.5, w_, ALU.is_ge, ALU.mult, [Rb], [Rb])
                        tt("dve", lo[:], lo[:], cnt[:], ALU.add, [Rb], [Rb])
                    dg = sb(st, "dg", [N_EXP, N_EXP]); Rdg = Res()
                    ts("dve", dg[:], idf[0:N_EXP, 0:N_EXP], lo[:, 0:1], None, ALU.mult, None, [RC, Rb], [Rdg])
                    p, Rp = npw()
                    mm(p[:, 0:N_EXP], ones_f[0:N_EXP, :], dg[:], True, True, [RC, Rdg], [Rp])
                    thr = sb(st, "thr", [128, N_EXP]); Rthr = Res()
                    cp("dve", thr[:], p[:, 0:N_EXP], [Rp], [Rthr])
                    gate = sb(st, "gate", [128, NTT, N_EXP]); Rgate = Res()
                    for ti in range(NTT):
                        tt("dve", gate[:, ti, :], aff[:, ti, :], thr[:], ALU.is_ge, [Raff, Rthr], [Rgate])
                    tt("dve", gate[:], gate[:], aff[:], ALU.mult, [Rgate, Raff], [Rgate])
                    NC16 = NTT * N_EXP
                    maskt = sb(st, "maskt", [128, NTT, N_EXP]); Rmk = Res()
                    ts("dve", maskt[:], gate[:], 0.0, None, ALU.is_gt, None, [Rgate], [Rmk])
                    pwi_, Rpwi = npw()
                    mm(pwi_[:, 0:NC16], triu[:], maskt[:].rearrange("p t e -> p (t e)"), True, True, [RC, Rmk], [Rpwi])
                    pto, Rpto = npw()
                    mm(pto[:, 0:NC16], ones_f[:], maskt[:].rearrange("p t e -> p (t e)"), True, True, [RC, Rmk], [Rpto])
                    tot = sb(st, "tot", [128, NTT, N_EXP]); Rtot = Res()
                    cp("act", tot[:].rearrange("p t e -> p (t e)"), pto[:, 0:NC16], [Rpto], [Rtot])
                    off = sb(st, "off", [128, NTT, N_EXP]); Roff = Res()
                    memset("dve", off[:, 0, :], 0.0, [Roff])
                    for ti in range(1, NTT):
                        tt("dve", off[:, ti, :], off[:, ti - 1, :], tot[:, ti - 1, :], ALU.add, [Roff, Rtot], [Roff])
                    rank = sb(st, "rank", [128, NTT, N_EXP]); Rrank = Res()
                    tt("dve", rank[:].rearrange("p t e -> p (t e)"), pwi_[:, 0:NC16], off[:].rearrange("p t e -> p (t e)"), ALU.add, [Rpwi, Roff], [Rrank])
                    valid = sb(st, "valid", [128, NTT, N_EXP]); Rvalid = Res()
                    ts("dve", valid[:], rank[:], cap - 0.5, None, ALU.is_lt, None, [Rrank], [Rvalid])
                    tt("dve", valid[:], valid[:], maskt[:], ALU.mult, [Rvalid, Rmk], [Rvalid])
                    tt("dve", gate[:], gate[:], valid[:], ALU.mult, [Rgate, Rvalid], [Rgate])
                    store("act", q["GATE"].rearrange("(t p) e -> p t e", p=128), gate[:], R=[Rgate], W=[q["R"]["GATE"]])
                    ts("dve", rank[:], rank[:], -60000.0, None, ALU.add, None, [Rrank], [Rrank])
                    tt("dve", rank[:], rank[:], valid[:], ALU.mult, [Rrank, Rvalid], [Rrank])
                    ts("dve", rank[:], rank[:], 60000.0, None, ALU.add, None, [Rrank], [Rrank])
                    idxi = sb(st, "idxi", [128, NTT, N_EXP], mybir.dt.int32); Ridx = Res()
                    cp("dve", idxi[:], rank[:], [Rrank], [Ridx])
                    store("act", q["IDX"].rearrange("(t p) e -> p t e", p=128), idxi[:], R=[Ridx], W=[q["R"]["IDX"]])
                S.barrier()

            for q in act_seqs:
                S.reg_vals.add(2 * q["T"] // N_EXP - 1)
            with ExitStack() as st:
                wg = [sb(st, "wg%d" % i, [128, KT, D], BF16) for i in range(2)]
                wu = [sb(st, "wu%d" % i, [128, KT, D], BF16) for i in range(2)]
                wd = [sb(st, "wd%d" % i, [128, KT, D], BF16) for i in range(2)]
                Rwe = [Res(), Res()]
                hr = [sb(st, "hr%d" % i, [128, 4, D], BF16) for i in range(2)]; Rhr = [Res(), Res()]
                hTe = [sb(st, "hTe%d" % i, [128, KT, 512], BF16) for i in range(2)]; RhTe = [Res(), Res()]
                aT = sb(st, "aT", [128, KT, 512], BF16); RaT = Res()
                sg = [sb(st, "sg%d" % i, [128, 512]) for i in range(2)]; Rsg = [Res(), Res()]
                yo = [sb(st, "yo%d" % i, [128, D]) for i in range(3)]; Ryo = [Res() for _ in range(3)]
                PE_ = [ps(st, "pe%d" % i) for i in range(6)]; RPE = [Res() for _ in range(6)]
                PTb = [ps(st, "ptb%d" % i, [128, 512], BF16) for i in range(2)]; RPTb = [Res(), Res()]
                pei = [0]

                def npe():
                    i = pei[0] % 6
                    pei[0] += 1
                    return PE_[i], RPE[i]
                cnt_h = 0
                cnt_y = 0
                hrow = [sb(st, "hrow%d" % i, [128, D], BF16) for i in range(8)]; Rhrow = [Res() for _ in range(8)]
                ixs = {}
                for q in act_seqs:
                    NTT = q["T"] // 128
                    ix = sb(st, "ixs_" + q["name"], [128, NTT * N_EXP], mybir.dt.int32); Rix = Res()
                    load("sp", ix[:].rearrange("p (t e) -> p t e", e=N_EXP), q["IDX"].rearrange("(t p) e -> p t e", p=128), R=[q["R"]["IDX"]], W=[Rix])
                    ixs[q["name"]] = (ix, Rix)
                sci = [0]

                def scatter(ex):
                    for q in act_seqs:
                        NTT = q["T"] // 128
                        cap = 2 * q["T"] // N_EXP
                        ix, Rix = ixs[q["name"]]
                        for ti in range(NTT):
                            b = sci[0] % 8
                            sci[0] += 1
                            load("sp", hrow[b][:], q["H2"][ti * 128:(ti + 1) * 128, :], R=[q["R"]["H2"]], W=[Rhrow[b]])
                            S.dma("pool", (lambda o_, ia, i_, cb: lambda e: e.indirect_dma_start(
                                out=o_, out_offset=bass.IndirectOffsetOnAxis(ap=ia, axis=0), in_=i_, in_offset=None,
                                bounds_check=S.regs[cb], oob_is_err=False))(q["XG"][ex][:, :], ix[:, ti * N_EXP + ex: ti * N_EXP + ex + 1], hrow[b][:], cap - 1),
                                reads=[Rhrow[b], Rix], writes=[q["RXG"][ex]])

                def wload(ex):
                    wi = ex % 2
                    for kk in range(KT):
                        load("pool", wg[wi][:, kk, :], w_gate[l, ex, kk * 128:(kk + 1) * 128, :], W=[Rwe[wi]])
                        load("pool", wu[wi][:, kk, :], w_up[l, ex, kk * 128:(kk + 1) * 128, :], W=[Rwe[wi]])
                        load("pool", wd[wi][:, kk, :], w_down[l, ex, kk * 128:(kk + 1) * 128, :], W=[Rwe[wi]])
                scatter(0); wload(0)
                for ex in range(N_EXP):
                    wi = ex % 2
                    if ex + 1 < N_EXP:
                        scatter(ex + 1); wload(ex + 1)
                    for q in act_seqs:
                        cap = 2 * q["T"] // N_EXP
                        rows = min(128, cap)
                        NSL = cap // rows
                        bi = cnt_h % 2
                        cnt_h += 1
                        load("sp", hr[bi][0:rows, 0:NSL, :], q["XG"][ex][0:cap, :].rearrange("(t p) d -> p t d", p=rows), R=[q["RXG"][ex]], W=[Rhr[bi]])
                        for kk in range(KT):
                            pb_ = kk % 2
                            for tl in range(NSL):
                                tr(PTb[pb_][:, tl * rows:(tl + 1) * rows], hr[bi][0:rows, tl, kk * 128:(kk + 1) * 128], idb[0:rows, 0:rows], [Rhr[bi], RC], [RPTb[pb_]])
                            cp("dve" if kk % 2 else "act", hTe[bi][:, kk, 0:cap], PTb[pb_][:, 0:cap], [RPTb[pb_]], [RhTe[bi]])
                        for f in range(KT):
                            pa, Rpa = npe()
                            for kk in range(KT):
                                mm(pa[:, 0:cap], wg[wi][:, kk, f * 128:(f + 1) * 128], hTe[bi][:, kk, 0:cap], kk == 0, kk == KT - 1, [Rwe[wi], RhTe[bi]], [Rpa])
                            pu, Rpu = npe()
                            for kk in range(KT):
                                mm(pu[:, 0:cap], wu[wi][:, kk, f * 128:(f + 1) * 128], hTe[bi][:, kk, 0:cap], kk == 0, kk == KT - 1, [Rwe[wi], RhTe[bi]], [Rpu])
                            si_ = f % 2
                            act(sg[si_][:, 0:cap], pa[:, 0:cap], AF.Silu, [Rpa], [Rsg[si_]])
                            tt("dve", aT[:, f, 0:cap], pu[:, 0:cap], sg[si_][:, 0:cap], ALU.mult, [Rpu, Rsg[si_]], [RaT])
                        for tl in range(NSL):
                            yi = cnt_y % 3
                            cnt_y += 1
                            for half in range(2):
                                py, Rpy = npe()
                                for f in range(KT):
                                    mm(py[0:rows, :], aT[:, f, tl * rows:(tl + 1) * rows], wd[wi][:, f, half * 512:(half + 1) * 512], f == 0, f == KT - 1, [RaT, Rwe[wi]], [Rpy])
                                cp("act" if half else "dve", yo[yi][0:rows, half * 512:(half + 1) * 512], py[0:rows, :], [Rpy], [Ryo[yi]])
                            store("act", q["Y"][ex][tl * rows:(tl + 1) * rows, :], yo[yi][0:rows, :], R=[Ryo[yi]], W=[q["RY"][ex]])
            S.barrier()

            with ExitStack() as st:
                gb = [sb(st, "gb%d" % i, [128, D]) for i in range(4)]; Rgb = [Res() for _ in range(4)]
                for i in range(4):
                    memset("dve" if i % 2 else "pool", gb[i][:], 0.0, [Rgb[i]])
                acc = [sb(st, "facc%d" % i, [128, D]) for i in range(2)]; Racc_ = [Res(), Res()]
                gi = 0
                ai = 0
                for q in act_seqs:
                    NTT = q["T"] // 128
                    cap = 2 * q["T"] // N_EXP
                    ix = sb(st, "ixg_" + q["name"], [128, NTT * N_EXP], mybir.dt.int32); Rix = Res()
                    load("sp", ix[:].rearrange("p (t e) -> p t e", e=N_EXP), q["IDX"].rearrange("(t p) e -> p t e", p=128), R=[q["R"]["IDX"]], W=[Rix])
                    gt_ = sb(st, "gtg_" + q["name"], [128, NTT, N_EXP]); Rgt_ = Res()
                    load("sp", gt_[:], q["GATE"].rearrange("(t p) e -> p t e", p=128), R=[q["R"]["GATE"]], W=[Rgt_])
                    for ti in range(NTT):
                        a = ai % 2
                        ai += 1
                        for ex in range(N_EXP):
                            b = gi % 4
                            gi += 1
                            S.dma("pool", (lambda o_, ia, i_, cb: lambda e: e.indirect_dma_start(
                                out=o_, out_offset=None, in_=i_, in_offset=bass.IndirectOffsetOnAxis(ap=ia, axis=0),
                                bounds_check=S.regs[cb], oob_is_err=False))(gb[b][:], ix[:, ti * N_EXP + ex: ti * N_EXP + ex + 1], q["Y"][ex][:, :], cap - 1),
                                reads=[q["RY"][ex], Rix], writes=[Rgb[b]])
                            if ex == 0:
                                ts("dve", acc[a][:], gb[b][:], gt_[:, ti, ex:ex + 1], None, ALU.mult, None, [Rgb[b], Rgt_], [Racc_[a]])
                            else:
                                stt("dve", acc[a][:], gb[b][:], gt_[:, ti, ex:ex + 1], acc[a][:], ALU.mult, ALU.add, [Rgb[b], Rgt_, Racc_[a]], [Racc_[a]])
                        store("act", q["ACC"][ti * 128:(ti + 1) * 128, :], acc[a][:], R=[Racc_[a]], W=[q["RACC"][ti]])
            S.barrier()

            for q in act_seqs:
                Tq = q["T"]
                j = q["j"]
                with ExitStack() as st:
                    RW = Res()
                    g2r = sb(st, "g2r", [128, D]); load("sp", g2r[:], MOD[l, j:j + 1, 5 * D:6 * D].broadcast_to([128, D]), R=[RMOD], W=[RW])
                    fnr = sb(st, "fnr", [128, D]); load("sp", fnr[:], fng.rearrange("(o d) -> o d", o=1).broadcast_to([128, D]), W=[RW])
                    xa = [sb(st, "x7a%d" % i, [128, D]) for i in range(2)]; Rxa = [Res(), Res()]
                    fa = [sb(st, "f7a%d" % i, [128, D]) for i in range(2)]; Rfa = [Res(), Res()]
                    junk = sb(st, "junk7", [128, D]); Rjunk = Res()
                    ss = sb(st, "ss7", [128, 1]); Rss = Res()
                    for ti in range(Tq // 128):
                        i = ti % 2
                        tok0 = ti * 128
                        load("sp", xa[i][:], q["XB"][tok0:tok0 + 128, :], R=[q["R"]["XB"]], W=[Rxa[i]])
                        load("sp", fa[i][:], q["ACC"][tok0:tok0 + 128, :], R=[q["RACC"][ti]], W=[Rfa[i]])
                        tt("dve", fa[i][:], fa[i][:], g2r[:], ALU.mult, [Rfa[i], RW], [Rfa[i]])
                        tt("dve", xa[i][:], xa[i][:], fa[i][:], ALU.add, [Rxa[i], Rfa[i]], [Rxa[i]])
                        if last:
                            act(junk[:], xa[i][:], AF.Square, [Rxa[i]], [Rjunk, Rss], accum_out=ss[:, 0:1])
                            rsqrt_ip("dve", ss[:], 1.0 / D, Rss)
                            stt("dve", xa[i][:], xa[i][:], ss[:, 0:1], fnr[:], ALU.mult, ALU.mult, [Rxa[i], Rss, RW], [Rxa[i]])
                            store("act", out[q["s"], tok0:tok0 + 128, :], xa[i][:], R=[Rxa[i]], W=[Res()])
                        else:
                            store("act", q["XA"][tok0:tok0 + 128, :], xa[i][:], R=[Rxa[i]], W=[q["R"]["XA"]])
                S.barrier()

        S.emit(top)
    return nc, S


_ROPE_PERM = np.array(list(range(8, 16)) + list(range(0, 8)) + list(range(24, 32)) + list(range(16, 24)))


def prep_shared(inp, T, TC, L):
    f = lambda a: np.ascontiguousarray(np.asarray(a, dtype=np.float32))
    inp = {kk: (np.asarray(v)[:L] if kk not in ("x", "c", "ctx", "c_ctx", "final_norm_g") else v) for kk, v in inp.items()}
    sh = {}
    w_in = f(inp["w_in"])
    sh["mod_w"] = f(inp["mod_w"]); sh["mod_b"] = f(inp["mod_b"])
    sh["n1gT"] = vecT(f(inp["norm1_g"])); sh["n2gT"] = vecT(f(inp["norm2_g"])); sh["n2g"] = f(inp["norm2_g"])
    sh["w_in"] = w_in
    sh["w_krs"] = np.ascontiguousarray(np.concatenate([np.zeros((L, D, 64), np.float32), w_in[:, :, OFF_KR + _ROPE_PERM]], axis=-1))
    sh["qagT"] = vecT(f(inp["q_a_g"])); sh["kvagT"] = vecT(f(inp["kv_a_g"]))
    wqb = f(inp["w_q_b"]).reshape(L, 256, NH, 96)
    sh["wq"] = np.ascontiguousarray(wqb.reshape(L, 256, NH * 96))
    sh["wqs"] = np.ascontiguousarray(np.concatenate([np.zeros((L, 256, NH, 64), np.float32), wqb[..., 64:96][..., _ROPE_PERM]], axis=-1).reshape(L, 256, NH * 96))
    wkv = f(inp["w_kv_b"]).reshape(L, 128, NH, 128)
    sh["wk"] = np.ascontiguousarray(wkv[..., 0:64].reshape(L, 128, NH * 64))
    sh["wv"] = np.ascontiguousarray(wkv[..., 64:128].reshape(L, 128, NH * 64))
    sh["dwwT"] = np.ascontiguousarray(f(inp["conv_dw_w"]).transpose(0, 2, 1).reshape(L, 2, 128, CONV_K).transpose(0, 2, 1, 3))
    sh["dwbT"] = vecT(f(inp["conv_dw_b"])); sh["clngT"] = vecT(f(inp["conv_ln_g"])); sh["clnbT"] = vecT(f(inp["conv_ln_b"]))
    sh["hsw"] = f(inp["hy_short_w"]); sh["hsb"] = f(inp["hy_short_b"])
    sh["hw1"] = f(inp["hy_w1"]); sh["hb1T"] = f(inp["hy_b1"])[..., None]
    sh["hw2"] = f(inp["hy_w2"]); sh["hb2T"] = f(inp["hy_b2"])[..., None]
    sh["hw3"] = f(inp["hy_w3"]); sh["hfrT"] = np.ascontiguousarray(f(inp["hy_sin_freq"]).transpose(0, 2, 1))
    sh["hdec"] = f(inp["hy_decay"]); sh["hskip"] = f(inp["hy_skip"])
    gnv = f(inp["group_norm_g"])
    sh["gnT"] = vecT(gnv); sh["gnaT"] = np.ascontiguousarray(gnv[:, 0:512].reshape(L, NH, 64).transpose(0, 2, 1)); sh["gn"] = gnv
    sh["w_out"] = f(inp["w_out"]); sh["router"] = f(inp["router_w"])
    sh["w_gate"] = f(inp["w_gate"]); sh["w_up"] = f(inp["w_up"]); sh["w_down"] = f(inp["w_down"])
    sh["fng"] = f(inp["final_norm_g"])
    sh["ident_f"] = np.eye(128, dtype=np.float32); sh["ident_b"] = _bf(np.eye(128, dtype=np.float32))
    sh["triu"] = np.ascontiguousarray(np.triu(np.ones((128, 128), np.float32), 1))
    rc, rs = rope_tables(T)
    sh["ropeC"] = rc; sh["ropeS"] = rs
    sh["sgn"] = (1.0 - 2.0 * (np.arange(128) % 2)).astype(np.float32)[:, None]
    for nm, TTT in (("L", T), ("C", TC)):
        hc = hyena_consts(TTT)
        for k in ("zT", "tposT", "maskT", "FW", "IV"):
            sh["hy_%s_%s" % (k, nm)] = hc[k]
    return sh


_CACHE = {}


def run(inp, n_cores, T, TC, L, NS=2, dbg=()):
    key = (T, TC, L, NS, tuple(dbg))
    import time as _t
    t0 = _t.time()
    if key not in _CACHE:
        _CACHE[key] = build_program(T, TC, L, NS, dbg)
    print("build %.1fs" % (_t.time() - t0))
    nc, S = _CACHE[key]
    sh = prep_shared(inp, T, TC, L)
    x = np.asarray(inp["x"], dtype=np.float32); ctx = np.asarray(inp["ctx"], dtype=np.float32)
    c = np.asarray(inp["c"], dtype=np.float32); cc = np.asarray(inp["c_ctx"], dtype=np.float32)
    in_maps = []
    for i in range(n_cores):
        m = dict(sh)
        m["x"] = np.ascontiguousarray(x[i * NS:(i + 1) * NS]); m["ctx"] = np.ascontiguousarray(ctx[i * NS:(i + 1) * NS])
        c3 = np.concatenate([c[i * NS:(i + 1) * NS], cc[None, :]], axis=0)
        m["c3T"] = np.ascontiguousarray(c3.reshape(3, D // 128, 128).transpose(2, 1, 0))
        in_maps.append(m)
    import time as _t
    t0 = _t.time()
    res = run_bass_kernel_spmd(nc, in_maps, core_ids=list(range(n_cores)))
    print("spmd launch wall %.1fs" % (_t.time() - t0))
    return res


def kernel(**inputs):
    res = run(inputs, 8, 4096, 256, 2)
    return np.concatenate([np.asarray(r["out"]) for r in res.results], axis=0).astype(np.float32)
```

```python
import math
import sys
from contextlib import ExitStack
import numpy as np
import ml_dtypes
import concourse.bass as bass
import concourse.mybir as mybir
from concourse.bass_utils import run_bass_kernel_spmd

F32 = mybir.dt.float32
BF16 = mybir.dt.bfloat16
AF = mybir.ActivationFunctionType
ALU = mybir.AluOpType
AX = mybir.AxisListType

SAME_ENGINE_SYNC = True
NDMA_SLOTS = 24

D = 1024
NH = 8
EPS = 1e-6
N_EXP = 16
GRID_W = 64
OFF_KV = 256
OFF_KR = 384
OFF_CONV = 416
OFF_HY = 928
IN_COLS = 1696
CONV_K = 31


class Res:
    __slots__ = ("name", "w", "r")

    def __init__(self, name=""):
        self.name = name
        self.w = None
        self.r = []


class Op:
    __slots__ = ("eng", "fn", "deps", "is_dma", "signals", "sem", "sigval", "clock", "waits", "line")

    def __init__(self, eng, fn, is_dma):
        f = sys._getframe(2)
        self.line = (f.f_lineno, f.f_back.f_lineno if f.f_back else 0)
        self.eng = eng
        self.fn = fn
        self.is_dma = is_dma
        self.deps = []
        self.signals = is_dma
        self.sem = None
        self.sigval = 0
        self.clock = None
        self.waits = []


class Sched:
    ENGS = ("pe", "act", "dve", "pool", "sp")

    def __init__(self, nc):
        self.nc = nc
        self.ops = []
        self.last = {}
        self.pending_dma = []
        self.reg_vals = set()
        self.regs = {}

    def _track(self, op, reads, writes):
        deps = []
        for r in reads:
            if r.w is not None:
                deps.append(r.w)
            if op.is_dma:
                r.r.append(op)
            else:
                r.r = [x for x in r.r if x.is_dma or x.eng != op.eng]
                r.r.append(op)
        for w in writes:
            if w.w is not None:
                deps.append(w.w)
            deps.extend(w.r)
            w.w = op
            w.r = []
        seen = set()
        for d in deps:
            if d is op or id(d) in seen:
                continue
            seen.add(id(d))
            op.deps.append(d)

    def op(self, eng, fn, reads=(), writes=()):
        o = Op(eng, fn, False)
        self._track(o, reads, writes)
        self.ops.append(o)
        self.last[eng] = o
        return o

    def dma(self, eng, fn, reads=(), writes=()):
        o = Op(eng, fn, True)
        self._track(o, reads, writes)
        self.ops.append(o)
        self.pending_dma.append(o)
        return o

    def barrier(self):
        deps = [o for o in self.last.values()] + list(self.pending_dma)
        self.pending_dma = []
        for e in self.ENGS:
            o = Op(e, None, False)
            o.deps = [d for d in deps]
            self.ops.append(o)

    def emit(self, stack):
        nc = self.nc
        for o in self.ops:
            for d in o.deps:
                if d.is_dma:
                    continue
                if d.eng != o.eng or o.is_dma:
                    d.signals = True
                elif SAME_ENGINE_SYNC and o.eng != "pe":
                    d.signals = True
        esem = {e: stack.enter_context(nc.semaphore("s_" + e)) for e in self.ENGS}
        ecount = {e: 0 for e in self.ENGS}
        clock = {e: {} for e in self.ENGS}
        slot_last = {}
        dcount = {e: 0 for e in self.ENGS}
        dsem = {}
        for e in sorted({o.eng for o in self.ops if o.is_dma}):
            dsem[e] = [stack.enter_context(nc.semaphore("d_%s_%d" % (e, i))) for i in range(NDMA_SLOTS)]
        per_eng = {e: [] for e in self.ENGS}
        nwaits = 0
        for o in self.ops:
            E = o.eng
            ck = clock[E]

            def need(d):
                nonlocal nwaits
                key = d.sem.num if d.is_dma else d.eng
                if not d.is_dma and d.eng == E and not o.is_dma:
                    if E == "pe" or not SAME_ENGINE_SYNC:
                        return
                if ck.get(key, 0) >= d.sigval:
                    return
                o.waits.append((d.sem, d.sigval))
                nwaits += 1
                for k, v in d.clock.items():
                    if ck.get(k, 0) < v:
                        ck[k] = v

            for d in o.deps:
                need(d)
            if o.is_dma:
                i = dcount[E]
                dcount[E] += 1
                s = i % NDMA_SLOTS
                prev = slot_last.get((E, s))
                if prev is not None:
                    need(prev)
                o.sem = dsem[E][s]
                o.sigval = (prev.sigval if prev is not None else 0) + 16
                slot_last[(E, s)] = o
                o.clock = dict(ck)
                o.clock[o.sem.num] = o.sigval
            else:
                o.sem = esem[E]
                if o.signals and o.fn is not None:
                    ecount[E] += 1
                o.sigval = ecount[E]
                o.clock = dict(ck)
                o.clock[E] = o.sigval
            per_eng[E].append(o)
        self.stats = dict(n_ops=len(self.ops), n_waits=nwaits, counts=dict(ecount),
                          per_eng={e: len(v) for e, v in per_eng.items()})
        final_waits = []
        for e in self.ENGS:
            if ecount[e] > 0:
                final_waits.append((esem[e], ecount[e]))
        for (e, s), o in slot_last.items():
            final_waits.append((o.sem, o.sigval))
        block = stack.enter_context(nc.Block())
        engobj = {"pe": "tensor", "act": "scalar", "dve": "vector", "pool": "gpsimd", "sp": "sync"}

        def make(e):
            lst = per_eng[e]

            def body(eng):
                if e == "pool":
                    for val in sorted(self.reg_vals):
                        r = eng.alloc_register("bc%d" % val)
                        eng.reg_mov(r, val)
                        self.regs[val] = r
                for o in lst:
                    for (sem, val) in o.waits:
                        eng.wait_ge(sem, val)
                    if o.fn is None:
                        continue
                    try:
                        ins = o.fn(eng)
                    except Exception:
                        print("EMIT FAIL at op created at lines", o.line, "eng", o.eng)
                        import traceback; traceback.print_exc()
                        raise
                    if o.is_dma:
                        ins.then_inc(o.sem, 16)
                    elif o.signals:
                        ins.then_inc(o.sem, 1)
                if e == "sp":
                    for (sem, val) in final_waits:
                        eng.wait_ge(sem, val)
            return body

        for e in self.ENGS:
            getattr(block, engobj[e])(make(e))


def _bf(a):
    return np.ascontiguousarray(a.astype(ml_dtypes.bfloat16))


def rope_tables(T):
    rows = T // GRID_W
    row = np.repeat(np.arange(rows, dtype=np.float32), GRID_W)
    col = np.tile(np.arange(GRID_W, dtype=np.float32), rows)
    half = 16
    inv = (10000.0 ** (-np.arange(0, half, 2, dtype=np.float32) / half)).astype(np.float32)
    ar = (row[:, None] * inv).astype(np.float32)
    ac = (col[:, None] * inv).astype(np.float32)
    cos = np.concatenate([np.cos(ar), np.cos(ar), np.cos(ac), np.cos(ac)], axis=1)
    sin = np.concatenate([-np.sin(ar), np.sin(ar), -np.sin(ac), np.sin(ac)], axis=1)
    return np.ascontiguousarray(cos.T.astype(np.float32)), np.ascontiguousarray(sin.T.astype(np.float32))


def hyena_consts(T):
    N = 2 * T
    t = np.linspace(0.0, 1.0, T, dtype=np.float32)
    w = (2.0 * math.pi * np.arange(T, dtype=np.float32) / T).astype(np.float32)
    f = np.linspace(1e-4, 15, 16, dtype=np.float32)
    fw = (w[:, None] * f[None, :]).astype(np.float32)
    z = np.concatenate([t[:, None], np.cos(fw), -np.sin(fw)], axis=-1).astype(np.float32)
    pos = np.zeros(N, dtype=np.int64)
    pos[:T] = np.arange(T)
    pos[T + 1:] = N - np.arange(T + 1, N)
    mask = np.ones(N, dtype=np.float32)
    mask[T] = 0.0
    zT = np.ascontiguousarray(z[pos].T)
    tpos = t[pos]
    NS = N // 128
    tposT = np.ascontiguousarray((-tpos).reshape(NS, 128).T)
    maskT = np.ascontiguousarray(mask.reshape(NS, 128).T)
    TT = T // 128
    NFT = TT + 1
    NFP = NFT * 128
    k = np.arange(NFP, dtype=np.float64)
    tt = np.arange(T, dtype=np.float64)
    ang = 2.0 * math.pi * np.outer(tt, k) / N
    valid = (k <= T).astype(np.float64)
    Fc = np.cos(ang) * valid
    Fs = -np.sin(ang) * valid
    FW = np.stack([Fc, Fs], 0).reshape(2, TT, 128, NFT, 128).transpose(3, 0, 2, 1, 4)
    FW = _bf(FW.reshape(NFT, 2, 128, TT * 128))
    ck = np.where((k == 0) | (k == T), 1.0, 2.0) * valid / N
    Ic = (np.cos(ang) * ck).T
    Is = (-np.sin(ang) * ck).T
    IV = np.stack([Ic, Is], 0).reshape(2, NFT, 128, TT, 128).transpose(3, 0, 2, 1, 4)
    IV = _bf(IV.reshape(TT, 2, 128, NFT * 128))
    return dict(zT=zT, tposT=tposT, maskT=maskT, FW=FW, IV=IV)


def vecT(v, width=128):
    n = v.shape[-1]
    return np.ascontiguousarray(v.reshape(v.shape[:-1] + (n // width, width)).swapaxes(-1, -2))


def build_program(T, TC, L, NS=2, dbg=()):
    nc = bass.Bass("TRN2", target_bir_lowering=False)
    S = Sched(nc)
    KT = D // 128

    def din(name, shape, dt=F32):
        return nc.dram_tensor(name, list(shape), dt, kind="ExternalInput").ap()

    def dscr(name, shape, dt=F32):
        return nc.dram_tensor(name, list(shape), dt, kind=("ExternalOutput" if dbg else "Internal")).ap()

    x_in = din("x", [NS, T, D])
    ctx_in = din("ctx", [NS, TC, D])
    c3T = din("c3T", [128, KT, 3])
    mod_w = din("mod_w", [L, D, 6 * D])
    mod_b = din("mod_b", [L, 6 * D])
    n1gT = din("n1gT", [L, 128, KT])
    n2gT = din("n2gT", [L, 128, KT])
    n2g = din("n2g", [L, D])
    w_in = din("w_in", [L, D, IN_COLS])
    w_krs = din("w_krs", [L, D, 96])
    qagT = din("qagT", [L, 128, 2])
    wq = din("wq", [L, 256, NH * 96])
    wqs = din("wqs", [L, 256, NH * 96])
    kvagT = din("kvagT", [L, 128, 1])
    wk = din("wk", [L, 128, NH * 64])
    wv = din("wv", [L, 128, NH * 64])
    dwwT = din("dwwT", [L, 128, 2, CONV_K])
    dwbT = din("dwbT", [L, 128, 2])
    clngT = din("clngT", [L, 128, 2])
    clnbT = din("clnbT", [L, 128, 2])
    hsw = din("hsw", [L, 3, 768])
    hsb = din("hsb", [L, 768])
    hw1 = din("hw1", [L, 33, 64])
    hb1T = din("hb1T", [L, 64, 1])
    hw2 = din("hw2", [L, 64, 64])
    hb2T = din("hb2T", [L, 64, 1])
    hw3 = din("hw3", [L, 64, 1024])
    hfrT = din("hfrT", [L, 64, 2])
    hdec = din("hdec", [L, 256])
    hskip = din("hskip", [L, 2, 256])
    gnT = din("gnT", [L, 128, 8])
    gnaT = din("gnaT", [L, 64, 8])
    gn = din("gn", [L, D])
    w_out = din("w_out", [L, D, D])
    router = din("router", [L, D, N_EXP])
    w_gate = din("w_gate", [L, N_EXP, D, D])
    w_up = din("w_up", [L, N_EXP, D, D])
    w_down = din("w_down", [L, N_EXP, D, D])
    fng = din("fng", [D])
    ident_f = din("ident_f", [128, 128])
    ident_b = din("ident_b", [128, 128], BF16)
    triu_in = din("triu", [128, 128])
    ropeC = din("ropeC", [32, T])
    ropeS = din("ropeS", [32, T])
    sgn_in = din("sgn", [128, 1])
    hyc = {}
    for (nm, TTT) in (("L", T), ("C", TC)):
        NSL = 2 * TTT // 128
        TTn = TTT // 128
        NFT = TTn + 1
        hyc[nm] = dict(
            zT=din("hy_zT_" + nm, [33, 2 * TTT]),
            tposT=din("hy_tposT_" + nm, [128, NSL]),
            maskT=din("hy_maskT_" + nm, [128, NSL]),
            FW=din("hy_FW_" + nm, [NFT, 2, 128, TTn * 128], BF16),
            IV=din("hy_IV_" + nm, [TTn, 2, 128, NFT * 128], BF16),
            T=TTT, TT=TTn, NFT=NFT, NSL=NSL)
    out = nc.dram_tensor("out", [NS, T, D], F32, kind="ExternalOutput").ap()

    NKEY = T + TC
    MOD = dscr("MOD", [L, 3, 6 * D])
    seqs = []
    for s in range(NS):
        seqs.append(dict(kind="L", s=s, j=s, T=T, rope=True, kbase=0, nkeys=NKEY, x0=x_in[s]))
    for s in range(NS):
        seqs.append(dict(kind="C", s=s, j=2, T=TC, rope=False, kbase=T, nkeys=TC, x0=ctx_in[s]))
    for q in seqs:
        nm = "%s%d" % (q["kind"], q["s"])
        q["name"] = nm
        q["XA"] = dscr("XA_" + nm, [q["T"], D])
        q["XB"] = dscr("XB_" + nm, [q["T"], D])
        q["QT"] = dscr("QT_" + nm, [NH, 96, q["T"]], BF16)
        q["ATT"] = dscr("ATT_" + nm, [NH, 64, q["T"]])
        q["Z"] = dscr("Z_" + nm, [q["T"], 768], BF16)
        q["MIXT"] = dscr("MIXT_" + nm, [512, q["T"]], BF16)
        q["ACC"] = dscr("ACC_" + nm, [q["T"], D])
        q["H2"] = dscr("H2_" + nm, [q["T"], D], BF16)
        q["GATE"] = dscr("GATE_" + nm, [q["T"], N_EXP])
        q["IDX"] = dscr("IDX_" + nm, [q["T"], N_EXP], mybir.dt.int32)
        capq = 2 * q["T"] // N_EXP
        q["XG"] = [dscr("XG_%s_%d" % (nm, e_), [max(capq, 128), D], BF16) for e_ in range(N_EXP)]
        q["Y"] = [dscr("Y_%s_%d" % (nm, e_), [max(capq, 128), D]) for e_ in range(N_EXP)]
        q["RXG"] = [Res() for _ in range(N_EXP)]
        q["RY"] = [Res() for _ in range(N_EXP)]
        q["R"] = {k: Res(nm + k) for k in ("XA", "XB", "QT", "ATT", "Z", "MIXT", "ACC", "H2", "GATE", "IDX")}
        q["RACC"] = [Res() for _ in range(q["T"] // 128)]
    KTd = [dscr("KT_%d" % s, [NH, 96, NKEY], BF16) for s in range(NS)]
    Vd = [dscr("V_%d" % s, [NKEY, NH * 64], BF16) for s in range(NS)]
    RKT = [Res("KT%d" % s) for s in range(NS)]
    RV = [Res("V%d" % s) for s in range(NS)]
    RMOD = Res("MOD")
    GSPEC = {nm: dscr("GSPEC_" + nm, [hyc[nm]["NFT"] * 128, 2, 2, 512], BF16) for nm in ("L", "C")}
    ULO = {nm: dscr("ULO_" + nm, [hyc[nm]["NFT"] * 128, 2, 512]) for nm in ("L", "C")}
    RGSPEC = {nm: Res("GSPEC" + nm) for nm in ("L", "C")}
    RULO = {nm: Res("ULO" + nm) for nm in ("L", "C")}
    RCONST = Res("const")
    dbg_out = {}

    def dbg_tensor(name, shape, dt=F32):
        t = nc.dram_tensor("dbg_" + name, list(shape), dt, kind="ExternalOutput").ap()
        dbg_out[name] = t
        return t

    uid = [0]

    def sb(st, name, shape, dt=F32):
        uid[0] += 1
        return st.enter_context(nc.sbuf_tensor("%s_%d" % (name, uid[0]), list(shape), dt))

    def ps(st, name, shape=(128, 512), dt=F32):
        uid[0] += 1
        return st.enter_context(nc.psum_tensor("%s_%d" % (name, uid[0]), list(shape), dt))

    def load(q, out_ap, in_ap, R=(), W=(), **kw):
        S.dma(q, lambda e: e.dma_start(out=out_ap, in_=in_ap, **kw), reads=list(R) + [RCONST], writes=W)

    def store(q, out_ap, in_ap, R=(), W=()):
        S.dma(q, lambda e: e.dma_start(out=out_ap, in_=in_ap), reads=R, writes=W)

    def mm(out_ap, lhsT, rhs, start, stop, R, W):
        S.op("pe", lambda e: e.matmul(out_ap, lhsT=lhsT, rhs=rhs, start=start, stop=stop), reads=R, writes=W)

    def tr(out_ap, in_ap, ident, R, W):
        S.op("pe", lambda e: e.transpose(out=out_ap, in_=in_ap, identity=ident), reads=R, writes=W)

    def act(out_ap, in_ap, func, R, W, **kw):
        S.op("act", lambda e: e.activation(out=out_ap, in_=in_ap, func=func, **kw), reads=R, writes=W)

    def tt(eng, out_ap, a, b, op, R, W):
        S.op(eng, lambda e: e.tensor_tensor(out=out_ap, in0=a, in1=b, op=op), reads=R, writes=W)

    def ts(eng, out_ap, a, s1, s2, op0, op1, R, W, **kw):
        if s2 is None:
            S.op(eng, lambda e: e.tensor_scalar(out=out_ap, in0=a, scalar1=s1, scalar2=None, op0=op0, **kw), reads=R, writes=W)
        else:
            S.op(eng, lambda e: e.tensor_scalar(out=out_ap, in0=a, scalar1=s1, scalar2=s2, op0=op0, op1=op1, **kw), reads=R, writes=W)

    def stt(eng, out_ap, a, sc, b, op0, op1, R, W):
        S.op(eng, lambda e: e.scalar_tensor_tensor(out=out_ap, in0=a, scalar=sc, in1=b, op0=op0, op1=op1), reads=R, writes=W)

    def cp(eng, out_ap, in_ap, R, W):
        if eng == "act":
            S.op("act", lambda e: e.copy(out=out_ap, in_=in_ap), reads=R, writes=W)
        else:
            S.op(eng, lambda e: e.tensor_copy(out=out_ap, in_=in_ap), reads=R, writes=W)

    def memset(eng, ap, val, W):
        S.op(eng, lambda e: e.memset(ap, val), writes=W)

    def recip(out_ap, in_ap, R, W):
        S.op("dve", lambda e: e.reciprocal(out=out_ap, in_=in_ap), reads=R, writes=W)

    def rmax(out_ap, in_ap, R, W):
        S.op("dve", lambda e: e.reduce_max(out=out_ap, in_=in_ap, axis=AX.X), reads=R, writes=W)

    def rsqrt_to(dst, src, scale, Rs, Rd, npart=128):
        act(dst, src, AF.Sqrt, list(Rs) + [RC], [Rd], bias=eps_t[0:npart, 0:1], scale=scale)
        recip(dst, dst, [Rd], [Rd])

    def rsqrt_ip(eng, ap, scale, R, npart=128):
        rsqrt_to(ap, ap, scale, [R], R, npart)

    with ExitStack() as top:
        idf = sb(top, "idf", [128, 128]); idb = sb(top, "idb", [128, 128], BF16)
        ones_f = sb(top, "ones_f", [128, 128]); sgn = sb(top, "sgn_sb", [128, 1])
        RC = Res("consts_sb")
        load("sp", idf[:], ident_f[:, :], W=[RC])
        load("sp", idb[:], ident_b[:, :], W=[RC])
        triu = sb(top, "triu_sb", [128, 128])
        load("sp", triu[:], triu_in[:, :], W=[RC])
        load("sp", sgn[:], sgn_in[:, :], W=[RC])
        memset("dve", ones_f[:], 1.0, [RC])
        eps_t = sb(top, "eps_t", [128, 1])
        memset("dve", eps_t[:], EPS, [RC])

        with ExitStack() as st:
            c3 = sb(st, "c3", [128, KT, 3]); sc3 = sb(st, "sc3", [128, KT, 3])
            Rc3 = Res()
            load("sp", c3[:], c3T[:, :, :], W=[Rc3])
            act(sc3[:], c3[:], AF.Silu, [Rc3], [Rc3])
            wbuf = [sb(st, "modw%d" % i, [128, KT, 512]) for i in range(2)]
            Rw = [Res(), Res()]
            bb = [sb(st, "modb%d" % i, [3, 512]) for i in range(2)]
            Rb = [Res(), Res()]
            pm = [ps(st, "pmod%d" % i) for i in range(2)]
            Rp = [Res(), Res()]
            it = 0
            for l in range(L):
                for cb in range(12):
                    i = it % 2
                    it += 1
                    load("sp", wbuf[i][:], mod_w[l, :, cb * 512:(cb + 1) * 512].rearrange("(k p) n -> p k n", p=128), W=[Rw[i]])
                    load("act", bb[i][:], mod_b[l:l + 1, cb * 512:(cb + 1) * 512].broadcast_to([3, 512]), W=[Rb[i]])
                    for k in range(KT):
                        mm(pm[i][0:3, :], sc3[:, k, :], wbuf[i][:, k, :], k == 0, k == KT - 1, [Rc3, Rw[i]], [Rp[i]])
                    tt("dve", bb[i][:], pm[i][0:3, :], bb[i][:], ALU.add, [Rp[i], Rb[i]], [Rb[i]])
                    store("act", MOD[l, :, cb * 512:(cb + 1) * 512], bb[i][:], R=[Rb[i]], W=[RMOD])
        S.barrier()

        for l in range(L):
            last = (l == L - 1)
            act_seqs = [q for q in seqs if not (last and q["kind"] == "C")]
            for q in seqs:
                q["Xin"] = q["x0"] if l == 0 else q["XA"]
                q["RXin"] = RCONST if l == 0 else q["R"]["XA"]

            for q in seqs:
                kv_only = last and q["kind"] == "C"
                Tq = q["T"]
                CT = min(512, Tq)
                NCH = Tq // CT
                NTL = CT // 128
                j = q["j"]
                with ExitStack() as st0, ExitStack() as st:
                    RW = Res("p1w")
                    dww = sb(st0, "dww", [128, 2, CONV_K]); dwb = sb(st0, "dwb", [128, 2])
                    clg = sb(st0, "clg", [128, 2]); clb = sb(st0, "clb", [128, 2]); gnc = sb(st0, "gnc", [128, 8])
                    yglu = sb(st0, "yglu", [128, 2, Tq + 30], BF16); Ryg = Res("yglu")
                    win = sb(st, "win", [128, KT, IN_COLS], BF16)
                    for k in range(KT):
                        load("pool", win[:, k, :], w_in[l, k * 128:(k + 1) * 128, :], W=[RW])
                    wkrs = sb(st, "wkrs", [128, KT, 96], BF16)
                    load("pool", wkrs[:], w_krs[l].rearrange("(k p) n -> p k n", p=128), W=[RW])
                    wq_sb = sb(st, "wq_sb", [128, 2, NH * 96], BF16)
                    load("pool", wq_sb[:], wq[l].rearrange("(k p) n -> p k n", p=128), W=[RW])
                    wqs_sb = sb(st, "wqs_sb", [128, 2, NH * 96], BF16)
                    load("pool", wqs_sb[:], wqs[l].rearrange("(k p) n -> p k n", p=128), W=[RW])
                    wk_sb = sb(st, "wk_sb", [128, NH * 64], BF16)
                    load("pool", wk_sb[:], wk[l], W=[RW])
                    wv_sb = sb(st, "wv_sb", [128, NH * 64], BF16)
                    load("pool", wv_sb[:], wv[l], W=[RW])
                    whj = sb(st, "whj", [128, 3, KT, 768], BF16)
                    hsw_rep = sb(st, "hsw_rep", [128, 3, 768])
                    hsb_rep = sb(st, "hsb_rep", [128, 768])
                    if not kv_only:
                        load("sp", hsw_rep[:], hsw[l:l + 1].broadcast_to([128, 3, 768]), W=[RW])
                        load("sp", hsb_rep[:], hsb[l:l + 1, :].broadcast_to([128, 768]), W=[RW])
                        for jj in range(3):
                            for k in range(KT):
                                tt("pool" if k % 2 else "dve", whj[:, jj, k, :], win[:, k, OFF_HY:OFF_HY + 768], hsw_rep[:, jj, :], ALU.mult, [RW], [RW])
                    A1 = sb(st, "A1", [128, KT]); B1 = sb(st, "B1", [128, KT]); tmpv = sb(st, "tmpv", [128, KT])
                    RS = Res("p1s")
                    load("sp", tmpv[:], MOD[l, j, D:2 * D].rearrange("(k p) -> p k", p=128), R=[RMOD], W=[RS], allow_slow_non_contiguous=True)
                    load("sp", B1[:], MOD[l, j, 0:D].rearrange("(k p) -> p k", p=128), R=[RMOD], W=[RS], allow_slow_non_contiguous=True)
                    load("sp", A1[:], n1gT[l], W=[RS])
                    stt("dve", A1[:], tmpv[:], 1.0, A1[:], ALU.add, ALU.mult, [RS], [RS])
                    qag = sb(st, "qag", [128, 2]); kvag = sb(st, "kvag", [128, 1])
                    load("sp", qag[:], qagT[l], W=[RS]); load("sp", kvag[:], kvagT[l], W=[RS])
                    load("sp", dww[:], dwwT[l], W=[RS]); load("sp", dwb[:], dwbT[l], W=[RS])
                    load("sp", clg[:], clngT[l], W=[RS]); load("sp", clb[:], clnbT[l], W=[RS])
                    load("sp", gnc[:], gnT[l], W=[RS])
                    hT = [sb(st, "hT%d" % i, [128, KT, CT + 2], BF16) for i in range(3)]
                    RhT = [Res("hT%d" % i) for i in range(3)]
                    xt = [sb(st, "xt%d" % i, [128, D]) for i in range(NTL)]
                    Rxt = [Res() for _ in range(NTL)]
                    ssq = sb(st, "ssq", [128, 4]); Rssq = Res()
                    junk = sb(st, "junk", [128, D]); Rjunk = Res()
                    PB = [ps(st, "pb%d" % i) for i in range(8)]
                    RPB = [Res("pb%d" % i) for i in range(8)]
                    pbi = [0]

                    def nextp():
                        i = pbi[0] % 8
                        pbi[0] += 1
                        return PB[i], RPB[i]
                    uq = sb(st, "uq", [128, 2, CT]); Ruq = Res()
                    sq = sb(st, "sq", [128, 2, CT]); Rsq = Res()
                    rstd = sb(st, "rstd", [128, CT]); Rrstd = Res()
                    qn = sb(st, "qn", [128, 2, CT], BF16); Rqn = Res()
                    ukv = sb(st, "ukv", [128, CT]); Rukv = Res()
                    kvn = sb(st, "kvn", [128, CT], BF16); Rkvn = Res()
                    rc = sb(st, "rc", [96, CT]); rs_ = sb(st, "rs", [96, CT]); Rrope = Res()
                    t32a = sb(st, "t32a", [96, CT]); t32b = sb(st, "t32b", [96, CT]); Rt32 = Res()
                    qh = [sb(st, "qh%d" % i, [96, CT], BF16) for i in range(2)]; Rqh = [Res(), Res()]
                    kh = [sb(st, "kh%d" % i, [96, CT], BF16) for i in range(2)]; Rkh = [Res(), Res()]
                    kr = sb(st, "kr", [96, CT], BF16); Rkr = Res()
                    vt = [sb(st, "vt%d" % i, [128, NH * 64], BF16) for i in range(2)]; Rvt = [Res(), Res()]
                    sig = sb(st, "sig", [128, CT]); Rsig = Res()
                    zt = [sb(st, "zt%d" % i, [128, 768], BF16) for i in range(2)]; Rzt = [Res(), Res()]
                    if not kv_only:
                        memset("dve", yglu[:], 0.0, [Ryg])
                    for i in range(3):
                        memset("pool", hT[i][:], 0.0, [RhT[i]])

                    def hy_proj(c):
                        b = c % 3
                        for tl in range(NTL):
                            zi = (c * NTL + tl) % 2
                            for half in range(2):
                                p, Rp = nextp()
                                n = 0
                                for jj in range(3):
                                    for k in range(KT):
                                        mm(p[:, 0:384], hT[b][:, k, tl * 128 + jj: tl * 128 + jj + 128],
                                           whj[:, jj, k, half * 384:(half + 1) * 384], n == 0, n == 3 * KT - 1,
                                           [RhT[b], RW], [Rp])
                                        n += 1
                                tt("dve", zt[zi][:, half * 384:(half + 1) * 384], p[:, 0:384],
                                   hsb_rep[:, half * 384:(half + 1) * 384], ALU.add, [Rp, RW], [Rzt[zi]])
                            tok0 = c * CT + tl * 128
                            store("act", q["Z"][tok0:tok0 + 128, :], zt[zi][:], R=[Rzt[zi]], W=[q["R"]["Z"]])

                    for c in range(NCH):
                        b = c % 3
                        for tl in range(NTL):
                            tok0 = c * CT + tl * 128
                            load("sp", xt[tl][:], q["Xin"][tok0:tok0 + 128, :], R=[q["RXin"]], W=[Rxt[tl]])
                            act(junk[:], xt[tl][:], AF.Square, [Rxt[tl]], [Rjunk, Rssq], accum_out=ssq[:, tl:tl + 1])
                        rsqrt_ip("dve", ssq[:, 0:NTL], 1.0 / D, Rssq)
                        for tl in range(NTL):
                            ts("dve", xt[tl][:], xt[tl][:], ssq[:, tl:tl + 1], None, ALU.mult, None, [Rxt[tl], Rssq], [Rxt[tl]])
                        for k in range(KT):
                            p, Rp = nextp()
                            for tl in range(NTL):
                                tr(p[:, tl * 128:(tl + 1) * 128], xt[tl][:, k * 128:(k + 1) * 128], idf[:], [Rxt[tl], RC], [Rp])
                            ts("dve", hT[b][:, k, 1:CT + 1], p[:, 0:CT], A1[:, k:k + 1], B1[:, k:k + 1], ALU.mult, ALU.add,
                               [Rp, RS], [RhT[b]])
                        if c > 0:
                            pb_ = (c - 1) % 3
                            cp("pool", hT[b][:, :, 0:1], hT[pb_][:, :, CT:CT + 1], [RhT[pb_]], [RhT[b]])
                            cp("pool", hT[pb_][:, :, CT + 1:CT + 2], hT[b][:, :, 1:2], [RhT[b]], [RhT[pb_]])
                        else:
                            memset("pool", hT[b][:, :, 0:1], 0.0, [RhT[b]])
                        if c == NCH - 1:
                            memset("pool", hT[b][:, :, CT + 1:CT + 2], 0.0, [RhT[b]])
                        hc = lambda k: hT[b][:, k, 1:CT + 1]
                        c0 = c * CT
                        if q["rope"]:
                            load("sp", rc[64:96, :], ropeC[:, c0:c0 + CT], W=[Rrope])
                            load("sp", rs_[64:96, :], ropeS[:, c0:c0 + CT], W=[Rrope])

                        def rope_combine(dst, pa, Rpa, pb2, Rpb2, Rdst):
                            if q["rope"]:
                                tt("dve", t32a[64:96, :], pa, rc[64:96, :], ALU.mult, [Rpa, Rrope], [Rt32])
                                tt("dve", t32b[64:96, :], pb2, rs_[64:96, :], ALU.mult, [Rpb2, Rrope], [Rt32])
                                tt("dve", dst, t32a[64:96, :], t32b[64:96, :], ALU.add, [Rt32], [Rdst])
                            else:
                                cp("dve", dst, pa, [Rpa], [Rdst])

                        if not kv_only:
                            for cc in range(2):
                                p, Rp = nextp()
                                for k in range(KT):
                                    mm(p[:, 0:CT], win[:, k, cc * 128:(cc + 1) * 128], hc(k), k == 0, k == KT - 1, [RW, RhT[b]], [Rp])
                                cp("act", uq[:, cc, :], p[:, 0:CT], [Rp], [Ruq])
                                act(sq[:, cc, :], p[:, 0:CT], AF.Square, [Rp], [Rsq])
                            p, Rp = nextp()
                            for cc in range(2):
                                mm(p[:, 0:CT], ones_f[:], sq[:, cc, :], cc == 0, cc == 1, [RC, Rsq], [Rp])
                            cp("dve", rstd[:], p[:, 0:CT], [Rp], [Rrstd])
                            rsqrt_ip("dve", rstd[:], 1.0 / 256, Rrstd)
                            for cc in range(2):
                                stt("dve", qn[:, cc, :], uq[:, cc, :], qag[:, cc:cc + 1], rstd[:], ALU.mult, ALU.mult, [Ruq, RS, Rrstd], [Rqn])
                            for h in range(NH):
                                hi = h % 2
                                p, Rp = nextp()
                                for cc in range(2):
                                    mm(p[0:96, 0:CT], wq_sb[:, cc, h * 96:(h + 1) * 96], qn[:, cc, :], cc == 0, cc == 1, [RW, Rqn], [Rp])
                                p2, Rp2 = nextp()
                                if q["rope"]:
                                    for cc in range(2):
                                        mm(p2[0:96, 0:CT], wqs_sb[:, cc, h * 96:(h + 1) * 96], qn[:, cc, :], cc == 0, cc == 1, [RW, Rqn], [Rp2])
                                cp("act", qh[hi][0:64, :], p[0:64, 0:CT], [Rp], [Rqh[hi]])
                                rope_combine(qh[hi][64:96, :], p[64:96, 0:CT], Rp, p2[64:96, 0:CT], Rp2, Rqh[hi])
                                store("act", q["QT"][h, :, c0:c0 + CT], qh[hi][:], R=[Rqh[hi]], W=[q["R"]["QT"]])
                        p, Rp = nextp()
                        for k in range(KT):
                            mm(p[:, 0:CT], win[:, k, OFF_KV:OFF_KV + 128], hc(k), k == 0, k == KT - 1, [RW, RhT[b]], [Rp])
                        cp("act", ukv[:], p[:, 0:CT], [Rp], [Rukv])
                        act(sq[:, 0, :], p[:, 0:CT], AF.Square, [Rp], [Rsq])
                        p, Rp = nextp()
                        mm(p[:, 0:CT], ones_f[:], sq[:, 0, :], True, True, [RC, Rsq], [Rp])
                        cp("dve", rstd[:], p[:, 0:CT], [Rp], [Rrstd])
                        rsqrt_ip("dve", rstd[:], 1.0 / 128, Rrstd)
                        stt("dve", kvn[:], ukv[:], kvag[:, 0:1], rstd[:], ALU.mult, ALU.mult, [Rukv, RS, Rrstd], [Rkvn])
                        p, Rp = nextp()
                        for k in range(KT):
                            mm(p[0:96, 0:CT], win[:, k, OFF_KR - 64:OFF_KR + 32], hc(k), k == 0, k == KT - 1, [RW, RhT[b]], [Rp])
                        p2, Rp2 = nextp()
                        if q["rope"]:
                            for k in range(KT):
                                mm(p2[0:96, 0:CT], wkrs[:, k, :], hc(k), k == 0, k == KT - 1, [RW, RhT[b]], [Rp2])
                        rope_combine(kr[64:96, :], p[64:96, 0:CT], Rp, p2[64:96, 0:CT], Rp2, Rkr)
                        kb = q["kbase"] + c0
                        s_ = q["s"]
                        for h in range(NH):
                            hi = h % 2
                            p, Rp = nextp()
                            mm(p[0:64, 0:CT], wk_sb[:, h * 64:(h + 1) * 64], kvn[:], True, True, [RW, Rkvn], [Rp])
                            cp("act", kh[hi][0:64, :], p[0:64, 0:CT], [Rp], [Rkh[hi]])
                            cp("pool", kh[hi][64:96, :], kr[64:96, :], [Rkr], [Rkh[hi]])
                            store("act", KTd[s_][h, :, kb:kb + CT], kh[hi][:], R=[Rkh[hi]], W=[RKT[s_]])
                        for tl in range(NTL):
                            vi = tl % 2
                            p, Rp = nextp()
                            mm(p[:, 0:512], kvn[:, tl * 128:(tl + 1) * 128], wv_sb[:], True, True, [Rkvn, RW], [Rp])
                            cp("act", vt[vi][:], p[:, 0:512], [Rp], [Rvt[vi]])
                            store("act", Vd[s_][kb + tl * 128: kb + tl * 128 + 128, :], vt[vi][:], R=[Rvt[vi]], W=[RV[s_]])
                        if not kv_only:
                            for cc in range(2):
                                pa, Rpa = nextp()
                                for k in range(KT):
                                    mm(pa[:, 0:CT], win[:, k, OFF_CONV + cc * 128: OFF_CONV + (cc + 1) * 128], hc(k), k == 0, k == KT - 1, [RW, RhT[b]], [Rpa])
                                pg, Rpg = nextp()
                                for k in range(KT):
                                    mm(pg[:, 0:CT], win[:, k, OFF_CONV + 256 + cc * 128: OFF_CONV + 256 + (cc + 1) * 128], hc(k), k == 0, k == KT - 1, [RW, RhT[b]], [Rpg])
                                act(sig[:], pg[:, 0:CT], AF.Sigmoid, [Rpg], [Rsig])
                                tt("dve", yglu[:, cc, 15 + c0: 15 + c0 + CT], pa[:, 0:CT], sig[:], ALU.mult, [Rpa, Rsig], [Ryg])
                            if c > 0:
                                hy_proj(c - 1)
                    if not kv_only:
                        hy_proj(NCH - 1)

                    st.close()
                    S.barrier()
                    if not kv_only:
                        st = st0
                        PB = [ps(st, "pc%d" % i) for i in range(6)]
                        RPB = [Res("pc%d" % i) for i in range(6)]
                        pci = [0]

                        def nextp():
                            i = pci[0] % 6
                            pci[0] += 1
                            return PB[i], RPB[i]
                        dgw = sb(st, "dgw", [128, 2, CONV_K, 128], BF16); Rdgw = Res()
                        for cc in range(2):
                            for jj in range(CONV_K):
                                ts("dve" if (jj % 2) else "pool", dgw[:, cc, jj, :], idb[:], dww[:, cc, jj:jj + 1], None, ALU.mult, None, [RC, RS], [Rdgw])
                        accs = sb(st, "cacc", [128, 2, CT]); Racc = [Res(), Res()]
                        mean = sb(st, "cmean", [128, CT]); var = sb(st, "cvar", [128, CT]); Rst = Res()
                        sq2 = sb(st, "csq", [128, 2, CT]); Rsq2 = Res()
                        zc = sb(st, "czc", [128, 2, CT]); Rzc = Res()
                        cvo = [sb(st, "cvo%d" % i, [128, 2, CT], BF16) for i in range(2)]; Rcvo = [Res(), Res()]
                        for c in range(NCH):
                            c0 = c * CT
                            for cc in range(2):
                                pcv, Rpcv = nextp()
                                for jj in range(CONV_K):
                                    mm(pcv[:, 0:CT], dgw[:, cc, jj, :], yglu[:, cc, c0 + jj:c0 + jj + CT], jj == 0, jj == CONV_K - 1, [Rdgw, Ryg], [Rpcv])
                                ts("dve", accs[:, cc, :], pcv[:, 0:CT], dwb[:, cc:cc + 1], None, ALU.add, None, [Rpcv, RS], [Racc[cc]])
                            for cc in range(2):
                                act(sq2[:, cc, :], accs[:, cc, :], AF.Square, [Racc[cc]], [Rsq2])
                            p1, Rp1 = nextp()
                            for cc in range(2):
                                mm(p1[:, 0:CT], ones_f[:], accs[:, cc, :], cc == 0, cc == 1, [RC, Racc[cc]], [Rp1])
                            p2, Rp2 = nextp()
                            for cc in range(2):
                                mm(p2[:, 0:CT], ones_f[:], sq2[:, cc, :], cc == 0, cc == 1, [RC, Rsq2], [Rp2])
                            ts("dve", mean[:], p1[:, 0:CT], 1.0 / 256, None, ALU.mult, None, [Rp1], [Rst])
                            tt("dve", var[:], mean[:], mean[:], ALU.mult, [Rst], [Rst])
                            stt("dve", var[:], p2[:, 0:CT], 1.0 / 256, var[:], ALU.mult, ALU.subtract, [Rp2, Rst], [Rst])
                            rsqrt_to(var[:], var[:], 1.0, [Rst], Rst)
                            for cc in range(2):
                                tt("dve", zc[:, cc, :], accs[:, cc, :], mean[:], ALU.subtract, [Racc[cc], Rst], [Rzc])
                                tt("dve", zc[:, cc, :], zc[:, cc, :], var[:], ALU.mult, [Rzc, Rst], [Rzc])
                                act(zc[:, cc, :], zc[:, cc, :], AF.Silu, [Rzc, RS], [Rzc], bias=clb[:, cc:cc + 1], scale=clg[:, cc:cc + 1])
                                act(sq2[:, cc, :], zc[:, cc, :], AF.Square, [Rzc], [Rsq2])
                            p3, Rp3 = nextp()
                            for cc in range(2):
                                mm(p3[:, 0:CT], ones_f[:], sq2[:, cc, :], cc == 0, cc == 1, [RC, Rsq2], [Rp3])
                            rsqrt_to(var[:], p3[:, 0:CT], 1.0 / 256, [Rp3], Rst)
                            oi = c % 2
                            for cc in range(2):
                                stt("dve", cvo[oi][:, cc, :], zc[:, cc, :], gnc[:, 4 + cc:5 + cc], var[:], ALU.mult, ALU.mult, [Rzc, RS, Rst], [Rcvo[oi]])
                                store("act", q["MIXT"][cc * 128:(cc + 1) * 128, c0:c0 + CT], cvo[oi][:, cc, :], R=[Rcvo[oi]], W=[q["R"]["MIXT"]])
                S.barrier()

            scale = 96.0 ** -0.5
            for q in act_seqs:
                Tq = q["T"]
                CT = min(512, Tq)
                NCH = Tq // CT
                NK = q["nkeys"]
                NKT = NK // 128
                s_ = q["s"]
                kb0 = q["kbase"] if q["kind"] == "C" else 0
                with ExitStack() as st:
                    ktb = [sb(st, "ktb%d" % i, [96, NK], BF16) for i in range(2)]; Rktb = [Res(), Res()]
                    qtb = [sb(st, "qtb%d" % i, [96, Tq], BF16) for i in range(2)]; Rqtb = [Res(), Res()]
                    vab = [sb(st, "vab%d" % i, [128, NKT, 65], BF16) for i in range(2)]; Rvab = [Res(), Res()]
                    e65 = sb(st, "e65", [65, 64]); Re = Res()
                    memset("dve", e65[:], 0.0, [Re])
                    memset("dve", e65[64:65, :], 1.0, [Re])
                    for i in range(2):
                        memset("dve", vab[i][:, :, 64:65], 1.0, [Rvab[i]])
                    PS_ = [ps(st, "pss%d" % i) for i in range(4)]; RPS = [Res() for _ in range(4)]
                    PO = [ps(st, "pso%d" % i) for i in range(2)]; RPO = [Res(), Res()]
                    PR = ps(st, "psr"); RPR = Res()
                    pt = [sb(st, "pt%d" % i, [128, CT], BF16) for i in range(4)]; Rpt = [Res() for _ in range(4)]
                    osb = [sb(st, "osb%d" % i, [65, CT]) for i in range(2)]; Rosb = [Res(), Res()]
                    rcp = sb(st, "rcp", [64, CT]); Rrcp = Res()
                    ao = [sb(st, "ao%d" % i, [64, CT]) for i in range(2)]; Rao = [Res(), Res()]
                    it = 0
                    io = 0
                    for h in range(NH):
                        hb = h % 2
                        load("sp", ktb[hb][:], KTd[s_][h, :, kb0:kb0 + NK], R=[RKT[s_]], W=[Rktb[hb]])
                        load("sp", qtb[hb][:], q["QT"][h], R=[q["R"]["QT"]], W=[Rqtb[hb]])
                        load("sp", vab[hb][:, :, 0:64], Vd[s_][kb0:kb0 + NK, h * 64:(h + 1) * 64].rearrange("(kt p) d -> p kt d", p=128),
                             R=[RV[s_]], W=[Rvab[hb]])
                        for c in range(NCH):
                            c0 = c * CT
                            oi = io % 2
                            io += 1
                            LA = 2
                            for kk in range(NKT + LA):
                                if kk < NKT:
                                    kt = kk
                                    i4 = kt % 4
                                    mm(PS_[i4][:, 0:CT], ktb[hb][:, kt * 128:(kt + 1) * 128], qtb[hb][:, c0:c0 + CT], True, True,
                                       [Rktb[hb], Rqtb[hb]], [RPS[i4]])
                                    act(pt[i4][:], PS_[i4][:, 0:CT], AF.Exp, [RPS[i4]], [Rpt[i4]], scale=scale)
                                if kk >= LA:
                                    kt = kk - LA
                                    i4 = kt % 4
                                    mm(PO[oi][0:65, 0:CT], vab[hb][:, kt, :], pt[i4][:], kt == 0, kt == NKT - 1, [Rvab[hb], Rpt[i4]], [RPO[oi]])
                            cp("dve", osb[oi][:], PO[oi][0:65, 0:CT], [RPO[oi]], [Rosb[oi]])
                            mm(PR[0:64, 0:CT], e65[:], osb[oi][:], True, True, [Re, Rosb[oi]], [RPR])
                            recip(rcp[:], PR[0:64, 0:CT], [RPR], [Rrcp])
                            tt("dve", ao[oi][:], osb[oi][0:64, :], rcp[:], ALU.mult, [Rosb[oi], Rrcp], [Rao[oi]])
                            store("act", q["ATT"][h, :, c0:c0 + CT], ao[oi][:], R=[Rao[oi]], W=[q["R"]["ATT"]])
                S.barrier()

            for nm in (("L", "C") if not last else ("L",)):
                hc_ = hyc[nm]
                Th = hc_["T"]; TTn = hc_["TT"]; NFT = hc_["NFT"]; NSL = hc_["NSL"]
                sq_ = [q for q in act_seqs if q["kind"] == nm]
                with ExitStack() as st:
                    RH = Res("hyw")
                    w1 = sb(st, "hw1", [33, 64]); w2 = sb(st, "hw2", [64, 64]); w3 = sb(st, "hw3", [64, 1024])
                    b1 = sb(st, "hb1", [64, 1]); b2 = sb(st, "hb2", [64, 1]); fr = sb(st, "hfr", [64, 2])
                    load("sp", w1[:], hw1[l], W=[RH]); load("sp", w2[:], hw2[l], W=[RH]); load("sp", w3[:], hw3[l], W=[RH])
                    load("sp", b1[:], hb1T[l], W=[RH]); load("sp", b2[:], hb2T[l], W=[RH]); load("sp", fr[:], hfrT[l], W=[RH])
                    fb = sb(st, "hfb", [64, 2])
                    tt("dve", fb[:, 0:1], b1[:], fr[:, 0:1], ALU.mult, [RH], [RH])
                    tt("dve", fb[:, 1:2], b2[:], fr[:, 1:2], ALU.mult, [RH], [RH])
                    ts("dve", fb[:], fb[:], 1.0 / (2 * math.pi), 8.5, ALU.mult, ALU.add, [RH], [RH])
                    ts("dve", fr[:], fr[:], 1.0 / (2 * math.pi), None, ALU.mult, None, [RH], [RH])
                    adec = sb(st, "adec", [128, 256])
                    load("sp", adec[:], hdec[l:l + 1, :].broadcast_to([128, 256]), W=[RH])
                    stt("dve", adec[:], adec[:], -1.0, adec[:], ALU.mult, ALU.max, [RH], [RH])
                    tpos = sb(st, "tpos", [128, NSL]); maskc = sb(st, "maskc", [128, NSL])
                    load("sp", tpos[:], hc_["tposT"], W=[RH]); load("sp", maskc[:], hc_["maskT"], W=[RH])
                    skp = sb(st, "skp", [128, 2, 2, 256])
                    for s2 in range(2):
                        load("sp", skp[:, :, s2, :], hskip[l:l + 1].broadcast_to([128, 2, 256]), W=[RH])
                    gnr = sb(st, "gnr", [128, 256])
                    load("sp", gnr[:], gn[l:l + 1, 768:1024].broadcast_to([128, 256]), W=[RH])
                    data = sb(st, "hdata", [128, TTn, 512], BF16); Rdata = Res("hdata")
                    fwb = [sb(st, "fwb%d" % i, [128, 2, TTn * 128], BF16) for i in range(2)]; Rfwb = [Res(), Res()]
                    stf = ExitStack()
                    hid2 = sb(stf, "hid2", [64, 2 * Th]); Rhid2 = Res()
                    PH = [ps(st, "ph%d" % i) for i in range(6)]; RPH = [Res() for _ in range(6)]
                    PSS = ps(st, "phss"); RPSS = Res()
                    phi = [0]

                    def nph():
                        i = phi[0] % 6
                        phi[0] += 1
                        return PH[i], RPH[i]
                    zb = [sb(stf, "zb%d" % i, [33, 512]) for i in range(2)]; Rzb = [Res(), Res()]
                    h1 = sb(stf, "h1", [64, 512]); Rh1 = Res()
                    NCB = (2 * Th) // 512

                    h1i = sb(stf, "h1i", [64, 512], mybir.dt.int32); h1k = sb(stf, "h1k", [64, 512])

                    def sin_layer(dst, src_ps, Rsrc, li, Rdst):
                        ts("dve", h1[:], src_ps, fr[:, li:li + 1], fb[:, li:li + 1], ALU.mult, ALU.add, [Rsrc, RH], [Rh1])
                        cp("dve", h1i[:], h1[:], [Rh1], [Rh1])
                        cp("dve", h1k[:], h1i[:], [Rh1], [Rh1])
                        tt("dve", h1[:], h1[:], h1k[:], ALU.subtract, [Rh1], [Rh1])
                        ts("dve", h1k[:], h1[:], 0.5, None, ALU.is_gt, None, [Rh1], [Rh1])
                        tt("dve", h1[:], h1[:], h1k[:], ALU.subtract, [Rh1], [Rh1])
                        ts("dve", h1[:], h1[:], -0.5, 0.5, ALU.max, ALU.min, [Rh1], [Rh1])
                        act(dst, h1[:], AF.Sin, [Rh1], [Rdst], scale=-6.283185)
                    hA = sb(stf, "hA", [64, 512]); RhA = Res()
                    for cb in range(NCB):
                        i = cb % 2
                        load("sp", zb[i][:], hc_["zT"][:, cb * 512:(cb + 1) * 512], W=[Rzb[i]])
                        p, Rp = nph()
                        mm(p[0:64, :], w1[:], zb[i][:], True, True, [RH, Rzb[i]], [Rp])
                        sin_layer(hA[:], p[0:64, :], Rp, 0, RhA)
                        p, Rp = nph()
                        mm(p[0:64, :], w2[:], hA[:], True, True, [RH, RhA], [Rp])
                        sin_layer(hid2[:, cb * 512:(cb + 1) * 512], p[0:64, :], Rp, 1, Rhid2)
                    dec = sb(stf, "hdecay", [128, 256]); Rdec = Res()
                    gsq = sb(stf, "gsq", [128, 512]); Rgsq = Res()
                    graw = sb(stf, "graw", [128, 512]); Rgraw = Res()
                    w3v = w3[:].rearrange("p (o d c) -> p o d c", o=2, d=2)
                    utile = sb(stf, "utile", [128, 2, 512]); Rut = Res()
                    gtile = [sb(stf, "gtile%d" % i, [128, 2, 2, 512], BF16) for i in range(2)]; Rgt = [Res(), Res()]
                    rn = sb(stf, "rnorm", [128, 512]); Rrn = Res()
                    fwi = [0]

                    def forward(consume):
                        for ft in range(NFT):
                            i = fwi[0] % 2
                            fwi[0] += 1
                            load("sp", fwb[i][:], hc_["FW"][ft].rearrange("r p n -> p r n"), W=[Rfwb[i]])
                            pr, Rpr = nph()
                            pi_, Rpi = nph()
                            for tc in range(TTn):
                                mm(pr[:], fwb[i][:, 0, tc * 128:(tc + 1) * 128], data[:, tc, :], tc == 0, tc == TTn - 1, [Rfwb[i], Rdata], [Rpr])
                            for tc in range(TTn):
                                mm(pi_[:], fwb[i][:, 1, tc * 128:(tc + 1) * 128], data[:, tc, :], tc == 0, tc == TTn - 1, [Rfwb[i], Rdata], [Rpi])
                            consume(ft, pr, Rpr, pi_, Rpi)

                    nss = [0]
                    for half in range(2):
                        for m in range(TTn):
                            slot = half * TTn + m
                            p, Rp = nph()
                            for o in range(2):
                                mm(p[:, o * 256:(o + 1) * 256], hid2[:, slot * 128:(slot + 1) * 128], w3v[:, o, half, :], True, True, [Rhid2, RH], [Rp])
                            act(dec[:], adec[:], AF.Exp, [RH], [Rdec], scale=tpos[:, slot:slot + 1])
                            for o in range(2):
                                stt("dve", graw[:, o * 256:(o + 1) * 256], p[:, o * 256:(o + 1) * 256], maskc[:, slot:slot + 1], dec[:], ALU.mult, ALU.mult,
                                    [Rp, RH, Rdec], [Rgraw])
                            cp("pool", data[:, m, :], graw[:], [Rgraw], [Rdata])
                            act(gsq[:], graw[:], AF.Square, [Rgraw], [Rgsq])
                            mm(PSS[:], ones_f[:], gsq[:], nss[0] == 0, nss[0] == 2 * TTn - 1, [RC, Rgsq], [RPSS])
                            nss[0] += 1
                        if half == 0:
                            def consume_lo(ft, pr, Rpr, pi_, Rpi):
                                cp("dve", utile[:, 0, :], pr[:], [Rpr], [Rut])
                                cp("act", utile[:, 1, :], pi_[:], [Rpi], [Rut])
                                store("act", ULO[nm][ft * 128:(ft + 1) * 128], utile[:], R=[Rut], W=[RULO[nm]])
                            forward(consume_lo)
                        else:
                            rsqrt_to(rn[:], PSS[:], 1.0, [RPSS], Rrn)

                            def consume_hi(ft, pr, Rpr, pi_, Rpi):
                                gi = ft % 2
                                load("sp", utile[:], ULO[nm][ft * 128:(ft + 1) * 128], R=[RULO[nm]], W=[Rut])
                                for ri, (pp, Rpp) in enumerate(((pr, Rpr), (pi_, Rpi))):
                                    stt("dve", utile[:, ri, :], pp[:], sgn[:, 0:1], utile[:, ri, :], ALU.mult, ALU.add, [Rpp, RC, Rut], [Rut])
                                    for o in range(2):
                                        for s2 in range(2):
                                            tt("dve", gtile[gi][:, o, ri, s2 * 256:(s2 + 1) * 256], utile[:, ri, o * 256:(o + 1) * 256],
                                               rn[:, o * 256:(o + 1) * 256], ALU.mult, [Rut, Rrn], [Rgt[gi]])
                                store("act", GSPEC[nm][ft * 128:(ft + 1) * 128], gtile[gi][:], R=[Rgt[gi]], W=[RGSPEC[nm]])
                            forward(consume_hi)

                    stf.close()
                    S.barrier()
                    spec = sb(st, "hspec", [128, NFT, 2, 512], BF16); Rspec = Res("hspec")
                    ivb = [sb(st, "ivb%d" % i, [128, 2, NFT * 128], BF16) for i in range(2)]; Rivb = [Res(), Res()]
                    for si, q in enumerate(sq_):
                        load("sp", data[:, :, si * 256:(si + 1) * 256], q["Z"][:, 0:256].rearrange("(tt p) c -> p tt c", p=128),
                             R=[q["R"]["Z"]], W=[Rdata])
                    gl = [sb(st, "gl%d" % i, [128, 2, 512], BF16) for i in range(2)]; Rgl = [Res(), Res()]
                    ta = sb(st, "hta", [128, 512]); tb = sb(st, "htb", [128, 512]); Rtab = Res()
                    xg = [sb(st, "xg%d" % i, [128, 512], BF16) for i in range(2)]; Rxg = [Res(), Res()]
                    yt = sb(st, "hyt", [128, 512]); Ryt = Res()
                    hss = sb(st, "hss", [128, 2]); Rhss = Res()
                    hyo = sb(st, "hyo", [128, 512], BF16); Rhyo = Res()
                    hyT = [sb(st, "hyT%d" % i, [128, 2, 2, 128], BF16) for i in range(2)]; RhyT = [Res(), Res()]
                    PT = ps(st, "phT", [128, 512], BF16); RPT = Res()
                    ivi = [0]
                    for o in range(2):
                        def consume_sig(ft, pr, Rpr, pi_, Rpi, o=o):
                            gi = ft % 2
                            load("sp", gl[gi][:], GSPEC[nm][ft * 128:(ft + 1) * 128, o], R=[RGSPEC[nm]], W=[Rgl[gi]])
                            tt("dve", ta[:], pr[:], gl[gi][:, 0, :], ALU.mult, [Rpr, Rgl[gi]], [Rtab])
                            tt("dve", tb[:], pi_[:], gl[gi][:, 1, :], ALU.mult, [Rpi, Rgl[gi]], [Rtab])
                            tt("dve", spec[:, ft, 0, :], ta[:], tb[:], ALU.subtract, [Rtab], [Rspec])
                            tt("dve", ta[:], pr[:], gl[gi][:, 1, :], ALU.mult, [Rpr, Rgl[gi]], [Rtab])
                            tt("dve", tb[:], pi_[:], gl[gi][:, 0, :], ALU.mult, [Rpi, Rgl[gi]], [Rtab])
                            tt("dve", spec[:, ft, 1, :], ta[:], tb[:], ALU.add, [Rtab], [Rspec])
                        forward(consume_sig)
                        for tti in range(TTn):
                            i = ivi[0] % 2
                            ivi[0] += 1
                            load("sp", ivb[i][:], hc_["IV"][tti].rearrange("r p n -> p r n"), W=[Rivb[i]])
                            xi = tti % 2
                            for si, q in enumerate(sq_):
                                load("act", xg[xi][:, si * 256:(si + 1) * 256], q["Z"][tti * 128:(tti + 1) * 128, 256 * (o + 1):256 * (o + 2)],
                                     R=[q["R"]["Z"]], W=[Rxg[xi]])
                            py, Rpy = nph()
                            n = 0
                            for ri in range(2):
                                for fc in range(NFT):
                                    mm(py[:], ivb[i][:, ri, fc * 128:(fc + 1) * 128], spec[:, fc, ri, :], n == 0, n == 2 * NFT - 1, [Rivb[i], Rspec], [Rpy])
                                    n += 1
                            tt("dve", yt[:], data[:, tti, :], skp[:, o].rearrange("p s c -> p (s c)"), ALU.mult, [Rdata, RH], [Ryt])
                            tt("dve", yt[:], yt[:], py[:], ALU.add, [Ryt, Rpy], [Ryt])
                            if o == 0:
                                tt("dve", data[:, tti, :], yt[:], xg[xi][:], ALU.mult, [Ryt, Rxg[xi]], [Rdata])
                            else:
                                tt("dve", yt[:], yt[:], xg[xi][:], ALU.mult, [Ryt, Rxg[xi]], [Ryt])
                                for si in range(len(sq_)):
                                    act(ta[:, 0:256], yt[:, si * 256:(si + 1) * 256], AF.Square, [Ryt], [Rtab, Rhss], accum_out=hss[:, si:si + 1])
                                rsqrt_ip("dve", hss[:, 0:len(sq_)], 1.0 / 256, Rhss)
                                for si in range(len(sq_)):
                                    stt("dve", hyo[:, si * 256:(si + 1) * 256], yt[:, si * 256:(si + 1) * 256], hss[:, si:si + 1], gnr[:], ALU.mult, ALU.mult,
                                        [Ryt, Rhss, RH], [Rhyo])
                                ti = tti % 2
                                for si in range(len(sq_)):
                                    for cc in range(2):
                                        tr(PT[:, (si * 2 + cc) * 128:(si * 2 + cc + 1) * 128], hyo[:, si * 256 + cc * 128: si * 256 + (cc + 1) * 128], idb[:], [Rhyo, RC], [RPT])
                                cp("dve", hyT[ti][:].rearrange("p s c t -> p (s c t)"), PT[:, :], [RPT], [RhyT[ti]])
                                for si, q in enumerate(sq_):
                                    for cc in range(2):
                                        store("act", q["MIXT"][256 + cc * 128: 256 + (cc + 1) * 128, tti * 128:(tti + 1) * 128], hyT[ti][:, si, cc, :],
                                              R=[RhyT[ti]], W=[q["R"]["MIXT"]])
                S.barrier()

            for q in act_seqs:
                Tq = q["T"]
                CT = min(512, Tq)
                NCH = Tq // CT
                NTL = CT // 128
                NTT = Tq // 128
                j = q["j"]
                cap = 2 * Tq // N_EXP
                with ExitStack() as st:
                    RW = Res("p5w")
                    woa = sb(st, "woa", [64, NH, D], BF16)
                    load("pool", woa[:], w_out[l, 0:512, :].rearrange("(h p) n -> p h n", p=64), W=[RW])
                    wob = sb(st, "wob", [128, 4, D], BF16)
                    load("pool", wob[:], w_out[l, 512:1024, :].rearrange("(k p) n -> p k n", p=128), W=[RW])
                    gna = sb(st, "gna", [64, NH]); load("sp", gna[:], gnaT[l], W=[RW])
                    g1r = sb(st, "g1r", [128, D]); load("sp", g1r[:], MOD[l, j:j + 1, 2 * D:3 * D].broadcast_to([128, D]), R=[RMOD], W=[RW])
                    A2r = sb(st, "A2r", [128, D]); B2r = sb(st, "B2r", [128, D]); n2r = sb(st, "n2r", [128, D])
                    load("sp", A2r[:], MOD[l, j:j + 1, 4 * D:5 * D].broadcast_to([128, D]), R=[RMOD], W=[RW])
                    load("sp", B2r[:], MOD[l, j:j + 1, 3 * D:4 * D].broadcast_to([128, D]), R=[RMOD], W=[RW])
                    load("sp", n2r[:], n2g[l:l + 1, :].broadcast_to([128, D]), W=[RW])
                    stt("dve", A2r[:], A2r[:], 1.0, n2r[:], ALU.add, ALU.mult, [RW], [RW])
                    rw = sb(st, "rw", [128, KT, N_EXP])
                    load("sp", rw[:], router[l].rearrange("(k p) e -> p k e", p=128), W=[RW])
                    att = sb(st, "att", [64, NH, CT]); Ratt = Res()
                    asq = sb(st, "asq", [64, NH, CT]); Rasq = Res()
                    ones64 = ones_f[0:64, :]
                    rstd = sb(st, "arstd", [128, CT]); Rrstd = Res()
                    attn = sb(st, "attn", [64, NH, CT], BF16); Rattn = Res()
                    mixb = sb(st, "mixb", [128, 4, CT], BF16); Rmixb = Res()
                    xt = [sb(st, "x5t%d" % i, [128, D]) for i in range(2)]; Rxt = [Res(), Res()]
                    ot = [sb(st, "o5t%d" % i, [128, D]) for i in range(2)]; Rot = [Res(), Res()]
                    junk = [sb(st, "junk5%d" % i, [128, D]) for i in range(2)]; Rjunk = [Res(), Res()]
                    ss2 = [sb(st, "ss2%d" % i, [128, 1]) for i in range(2)]; Rss2 = [Res(), Res()]
                    h2r = [sb(st, "h2r%d" % i, [128, D], BF16) for i in range(2)]; Rh2r = [Res(), Res()]
                    h2f = [sb(st, "h2f%d" % i, [128, D]) for i in range(2)]; Rh2f = [Res(), Res()]
                    h2T = [sb(st, "h2T%d" % i, [128, KT, 128]) for i in range(2)]; Rh2T = [Res(), Res()]
                    lg = [sb(st, "lg%d" % i, [128, N_EXP]) for i in range(2)]; Rlg = [Res(), Res()]
                    mx = [sb(st, "mx%d" % i, [128, 1]) for i in range(2)]; Rmx = [Res(), Res()]
                    aff = sb(st, "aff", [128, NTT, N_EXP]); Raff = Res("aff")
                    affT = sb(st, "affT", [N_EXP, Tq]); RaffT = Res()
                    PW = [ps(st, "pw%d" % i) for i in range(6)]; RPW = [Res() for _ in range(6)]
                    pwi = [0]

                    def npw():
                        i = pwi[0] % 6
                        pwi[0] += 1
                        return PW[i], RPW[i]
                    for c in range(NCH):
                        c0 = c * CT
                        for h in range(NH):
                            load("sp", att[:, h, :], q["ATT"][h, :, c0:c0 + CT], R=[q["R"]["ATT"]], W=[Ratt])
                        load("sp", mixb[:], q["MIXT"][:, c0:c0 + CT].rearrange("(k p) t -> p k t", p=128), R=[q["R"]["MIXT"]], W=[Rmixb])
                        act(asq[:], att[:], AF.Square, [Ratt], [Rasq])
                        p, Rp = npw()
                        for h in range(NH):
                            mm(p[:, 0:CT], ones64, asq[:, h, :], h == 0, h == NH - 1, [RC, Rasq], [Rp])
                        cp("dve", rstd[:], p[:, 0:CT], [Rp], [Rrstd])
                        rsqrt_ip("dve", rstd[:], 1.0 / 512, Rrstd)
                        for h in range(NH):
                            stt("dve", attn[:, h, :], att[:, h, :], gna[:, h:h + 1], rstd[0:64, :], ALU.mult, ALU.mult, [Ratt, RW, Rrstd], [Rattn])
                        for tl in range(NTL):
                            tok0 = c0 + tl * 128
                            tile_i = tok0 // 128
                            xi = tile_i % 2
                            load("sp", xt[xi][:], q["Xin"][tok0:tok0 + 128, :], R=[q["RXin"]], W=[Rxt[xi]])
                            for half in range(2):
                                p, Rp = npw()
                                n = 0
                                for h in range(NH):
                                    mm(p[:, :], attn[:, h, tl * 128:(tl + 1) * 128], woa[:, h, half * 512:(half + 1) * 512], n == 0, False, [Rattn, RW], [Rp])
                                    n += 1
                                for k in range(4):
                                    mm(p[:, :], mixb[:, k, tl * 128:(tl + 1) * 128], wob[:, k, half * 512:(half + 1) * 512], False, k == 3, [Rmixb, RW], [Rp])
                                tt("dve", ot[xi][:, half * 512:(half + 1) * 512], p[:, :], g1r[:, half * 512:(half + 1) * 512], ALU.mult, [Rp, RW], [Rot[xi]])
                            tt("dve", xt[xi][:], xt[xi][:], ot[xi][:], ALU.add, [Rxt[xi], Rot[xi]], [Rxt[xi]])
                            store("act", q["XB"][tok0:tok0 + 128, :], xt[xi][:], R=[Rxt[xi]], W=[q["R"]["XB"]])
                            act(junk[xi][:], xt[xi][:], AF.Square, [Rxt[xi]], [Rjunk[xi], Rss2[xi]], accum_out=ss2[xi][:, 0:1])
                            rsqrt_ip("dve", ss2[xi][:], 1.0 / D, Rss2[xi])
                            stt("dve", h2f[xi][:], xt[xi][:], ss2[xi][:, 0:1], A2r[:], ALU.mult, ALU.mult, [Rxt[xi], Rss2[xi], RW], [Rh2f[xi]])
                            tt("dve", h2f[xi][:], h2f[xi][:], B2r[:], ALU.add, [Rh2f[xi], RW], [Rh2f[xi]])
                            hi = tile_i % 2
                            cp("pool", h2r[hi][:], h2f[xi][:], [Rh2f[xi]], [Rh2r[hi]])
                            store("pool", q["H2"][tok0:tok0 + 128, :], h2r[hi][:], R=[Rh2r[hi]], W=[q["R"]["H2"]])
                            for k in range(KT):
                                if k % 4 == 0:
                                    p, Rp = npw()
                                tr(p[:, (k % 4) * 128:(k % 4 + 1) * 128], h2f[xi][:, k * 128:(k + 1) * 128], idf[:], [Rh2f[xi], RC], [Rp])
                                if k % 4 == 3:
                                    cp("act", h2T[xi][:, k - 3:k + 1, :].rearrange("p k t -> p (k t)"), p[:, :], [Rp], [Rh2T[xi]])
                            p, Rp = npw()
                            for k in range(KT):
                                mm(p[:, 0:N_EXP], h2T[xi][:, k, :], rw[:, k, :], k == 0, k == KT - 1, [Rh2T[xi], RW], [Rp])
                            cp("dve", lg[xi][:], p[:, 0:N_EXP], [Rp], [Rlg[xi]])
                            rmax(mx[xi][:], lg[xi][:], [Rlg[xi]], [Rmx[xi]])
                            ts("dve", mx[xi][:], mx[xi][:], -1.0, None, ALU.mult, None, [Rmx[xi]], [Rmx[xi]])
                            act(lg[xi][:], lg[xi][:], AF.Exp, [Rlg[xi], Rmx[xi]], [Rlg[xi], Rss2[xi]], bias=mx[xi][:, 0:1], accum_out=ss2[xi][:, 0:1])
                            recip(ss2[xi][:], ss2[xi][:], [Rss2[xi]], [Rss2[xi]])
                            ts("dve", aff[:, tile_i, :], lg[xi][:], ss2[xi][:, 0:1], None, ALU.mult, None, [Rlg[xi], Rss2[xi]], [Raff])
                            p, Rp = npw()
                            tr(p[0:N_EXP, 0:128], aff[:, tile_i, :], idf[:], [Raff, RC], [Rp])
                            cp("act", affT[:, tok0:tok0 + 128], p[0:N_EXP, 0:128], [Rp], [RaffT])
                    lo = sb(st, "lo", [N_EXP, 1]); mid = sb(st, "mid", [N_EXP, 1]); cnt = sb(st, "cnt", [N_EXP, 1]); Rb = Res()
                    cmpj = sb(st, "cmpj", [N_EXP, Tq]); Rcmp = Res()
                    memset("dve", lo[:], 0.0, [Rb])
                    for it_ in range(1, 29):
                        w_ = 2.0 ** (-it_)
                        ts("dve", mid[:], lo[:], w_, None, ALU.add, None, [Rb], [Rb])
                        ts("dve", cmpj[:], affT[:], mid[:, 0:1], 0.0, ALU.is_ge, ALU.add, [RaffT, Rb], [Rcmp, Rb], accum_out=cnt[:, 0:1])
                        ts("dve", cnt[:], cnt[:], cap - 0.5, w_, ALU.is_ge, ALU.mult, [Rb], [Rb])
                        tt("dve", lo[:], lo[:], cnt[:], ALU.add, [Rb], [Rb])
                    dg = sb(st, "dg", [N_EXP, N_EXP]); Rdg = Res()
                    ts("dve", dg[:], idf[0:N_EXP, 0:N_EXP], lo[:, 0:1], None, ALU.mult, None, [RC, Rb], [Rdg])
                    p, Rp = npw()
                    mm(p[:, 0:N_EXP], ones_f[0:N_EXP, :], dg[:], True, True, [RC, Rdg], [Rp])
                    thr = sb(st, "thr", [128, N_EXP]); Rthr = Res()
                    cp("dve", thr[:], p[:, 0:N_EXP], [Rp], [Rthr])
                    gate = sb(st, "gate", [128, NTT, N_EXP]); Rgate = Res()
                    for ti in range(NTT):
                        tt("dve", gate[:, ti, :], aff[:, ti, :], thr[:], ALU.is_ge, [Raff, Rthr], [Rgate])
                    tt("dve", gate[:], gate[:], aff[:], ALU.mult, [Rgate, Raff], [Rgate])
                    NC16 = NTT * N_EXP
                    maskt = sb(st, "maskt", [128, NTT, N_EXP]); Rmk = Res()
                    ts("dve", maskt[:], gate[:], 0.0, None, ALU.is_gt, None, [Rgate], [Rmk])
                    pwi_, Rpwi = npw()
                    mm(pwi_[:, 0:NC16], triu[:], maskt[:].rearrange("p t e -> p (t e)"), True, True, [RC, Rmk], [Rpwi])
                    pto, Rpto = npw()
                    mm(pto[:, 0:NC16], ones_f[:], maskt[:].rearrange("p t e -> p (t e)"), True, True, [RC, Rmk], [Rpto])
                    tot = sb(st, "tot", [128, NTT, N_EXP]); Rtot = Res()
                    cp("act", tot[:].rearrange("p t e -> p (t e)"), pto[:, 0:NC16], [Rpto], [Rtot])
                    off = sb(st, "off", [128, NTT, N_EXP]); Roff = Res()
                    memset("dve", off[:, 0, :], 0.0, [Roff])
                    for ti in range(1, NTT):
                        tt("dve", off[:, ti, :], off[:, ti - 1, :], tot[:, ti - 1, :], ALU.add, [Roff, Rtot], [Roff])
                    rank = sb(st, "rank", [128, NTT, N_EXP]); Rrank = Res()
                    tt("dve", rank[:].rearrange("p t e -> p (t e)"), pwi_[:, 0:NC16], off[:].rearrange("p t e -> p (t e)"), ALU.add, [Rpwi, Roff], [Rrank])
                    valid = sb(st, "valid", [128, NTT, N_EXP]); Rvalid = Res()
                    ts("dve", valid[:], rank[:], cap - 0.5, None, ALU.is_lt, None, [Rrank], [Rvalid])
                    tt("dve", valid[:], valid[:], maskt[:], ALU.mult, [Rvalid, Rmk], [Rvalid])
                    tt("dve", gate[:], gate[:], valid[:], ALU.mult, [Rgate, Rvalid], [Rgate])
                    store("act", q["GATE"].rearrange("(t p) e -> p t e", p=128), gate[:], R=[Rgate], W=[q["R"]["GATE"]])
                    ts("dve", rank[:], rank[:], -60000.0, None, ALU.add, None, [Rrank], [Rrank])
                    tt("dve", rank[:], rank[:], valid[:], ALU.mult, [Rrank, Rvalid], [Rrank])
                    ts("dve", rank[:], rank[:], 60000.0, None, ALU.add, None, [Rrank], [Rrank])
                    idxi = sb(st, "idxi", [128, NTT, N_EXP], mybir.dt.int32); Ridx = Res()
                    cp("dve", idxi[:], rank[:], [Rrank], [Ridx])
                    store("act", q["IDX"].rearrange("(t p) e -> p t e", p=128), idxi[:], R=[Ridx], W=[q["R"]["IDX"]])
                S.barrier()

            for q in act_seqs:
                S.reg_vals.add(2 * q["T"] // N_EXP - 1)
            with ExitStack() as st:
                hrow = [sb(st, "hrow%d" % i, [128, D], BF16) for i in range(4)]; Rhrow = [Res() for _ in range(4)]
                ci = 0
                for q in act_seqs:
                    NTT = q["T"] // 128
                    cap = 2 * q["T"] // N_EXP
                    ix = sb(st, "ixs_" + q["name"], [128, NTT * N_EXP], mybir.dt.int32); Rix = Res()
                    load("sp", ix[:].rearrange("p (t e) -> p t e", e=N_EXP), q["IDX"].rearrange("(t p) e -> p t e", p=128), R=[q["R"]["IDX"]], W=[Rix])
                    for ti in range(NTT):
                        b = ci % 4
                        ci += 1
                        load("sp", hrow[b][:], q["H2"][ti * 128:(ti + 1) * 128, :], R=[q["R"]["H2"]], W=[Rhrow[b]])
                        for ex in range(N_EXP):
                            S.dma("pool", (lambda o_, ia, i_, cb: lambda e: e.indirect_dma_start(
                                out=o_, out_offset=bass.IndirectOffsetOnAxis(ap=ia, axis=0), in_=i_, in_offset=None,
                                bounds_check=S.regs[cb], oob_is_err=False))(q["XG"][ex][:, :], ix[:, ti * N_EXP + ex: ti * N_EXP + ex + 1], hrow[b][:], cap - 1),
                                reads=[Rhrow[b], Rix], writes=[q["RXG"][ex]])
            S.barrier()

            with ExitStack() as st:
                wg = [sb(st, "wg%d" % i, [128, KT, D], BF16) for i in range(2)]
                wu = [sb(st, "wu%d" % i, [128, KT, D], BF16) for i in range(2)]
                wd = [sb(st, "wd%d" % i, [128, KT, D], BF16) for i in range(2)]
                Rwe = [Res(), Res()]
                hr = [sb(st, "hr%d" % i, [128, 4, D], BF16) for i in range(2)]; Rhr = [Res(), Res()]
                hTe = [sb(st, "hTe%d" % i, [128, KT, 512], BF16) for i in range(2)]; RhTe = [Res(), Res()]
                aT = sb(st, "aT", [128, KT, 512], BF16); RaT = Res()
                sg = [sb(st, "sg%d" % i, [128, 512]) for i in range(2)]; Rsg = [Res(), Res()]
                yo = [sb(st, "yo%d" % i, [128, D]) for i in range(3)]; Ryo = [Res() for _ in range(3)]
                PE_ = [ps(st, "pe%d" % i) for i in range(6)]; RPE = [Res() for _ in range(6)]
                PTb = [ps(st, "ptb%d" % i, [128, 512], BF16) for i in range(2)]; RPTb = [Res(), Res()]
                pei = [0]

                def npe():
                    i = pei[0] % 6
                    pei[0] += 1
                    return PE_[i], RPE[i]
                cnt_h = 0
                cnt_y = 0
                for ex in range(N_EXP):
                    wi = ex % 2
                    for kk in range(KT):
                        load("pool", wg[wi][:, kk, :], w_gate[l, ex, kk * 128:(kk + 1) * 128, :], W=[Rwe[wi]])
                        load("pool", wu[wi][:, kk, :], w_up[l, ex, kk * 128:(kk + 1) * 128, :], W=[Rwe[wi]])
                        load("pool", wd[wi][:, kk, :], w_down[l, ex, kk * 128:(kk + 1) * 128, :], W=[Rwe[wi]])
                    for q in act_seqs:
                        cap = 2 * q["T"] // N_EXP
                        rows = min(128, cap)
                        NSL = cap // rows
                        bi = cnt_h % 2
                        cnt_h += 1
                        load("sp", hr[bi][0:rows, 0:NSL, :], q["XG"][ex][0:cap, :].rearrange("(t p) d -> p t d", p=rows), R=[q["RXG"][ex]], W=[Rhr[bi]])
                        for kk in range(KT):
                            pb_ = kk % 2
                            for tl in range(NSL):
                                tr(PTb[pb_][:, tl * rows:(tl + 1) * rows], hr[bi][0:rows, tl, kk * 128:(kk + 1) * 128], idb[0:rows, 0:rows], [Rhr[bi], RC], [RPTb[pb_]])
                            cp("dve" if kk % 2 else "act", hTe[bi][:, kk, 0:cap], PTb[pb_][:, 0:cap], [RPTb[pb_]], [RhTe[bi]])
                        for f in range(KT):
                            pa, Rpa = npe()
                            for kk in range(KT):
                                mm(pa[:, 0:cap], wg[wi][:, kk, f * 128:(f + 1) * 128], hTe[bi][:, kk, 0:cap], kk == 0, kk == KT - 1, [Rwe[wi], RhTe[bi]], [Rpa])
                            pu, Rpu = npe()
                            for kk in range(KT):
                                mm(pu[:, 0:cap], wu[wi][:, kk, f * 128:(f + 1) * 128], hTe[bi][:, kk, 0:cap], kk == 0, kk == KT - 1, [Rwe[wi], RhTe[bi]], [Rpu])
                            si_ = f % 2
                            act(sg[si_][:, 0:cap], pa[:, 0:cap], AF.Silu, [Rpa], [Rsg[si_]])
                            tt("dve", aT[:, f, 0:cap], pu[:, 0:cap], sg[si_][:, 0:cap], ALU.mult, [Rpu, Rsg[si_]], [RaT])
                        for tl in range(NSL):
                            yi = cnt_y % 3
                            cnt_y += 1
                            for half in range(2):
                                py, Rpy = npe()
                                for f in range(KT):
                                    mm(py[0:rows, :], aT[:, f, tl * rows:(tl + 1) * rows], wd[wi][:, f, half * 512:(half + 1) * 512], f == 0, f == KT - 1, [RaT, Rwe[wi]], [Rpy])
                                cp("act" if half else "dve", yo[yi][0:rows, half * 512:(half + 1) * 512], py[0:rows, :], [Rpy], [Ryo[yi]])
                            store("act", q["Y"][ex][tl * rows:(tl + 1) * rows, :], yo[yi][0:rows, :], R=[Ryo[yi]], W=[q["RY"][ex]])
            S.barrier()

            with ExitStack() as st:
                gb = [sb(st, "gb%d" % i, [128, D]) for i in range(4)]; Rgb = [Res() for _ in range(4)]
                for i in range(4):
                    memset("dve" if i % 2 else "pool", gb[i][:], 0.0, [Rgb[i]])
                acc = [sb(st, "facc%d" % i, [128, D]) for i in range(2)]; Racc_ = [Res(), Res()]
                gi = 0
                ai = 0
                for q in act_seqs:
                    NTT = q["T"] // 128
                    cap = 2 * q["T"] // N_EXP
                    ix = sb(st, "ixg_" + q["name"], [128, NTT * N_EXP], mybir.dt.int32); Rix = Res()
                    load("sp", ix[:].rearrange("p (t e) -> p t e", e=N_EXP), q["IDX"].rearrange("(t p) e -> p t e", p=128), R=[q["R"]["IDX"]], W=[Rix])
                    gt_ = sb(st, "gtg_" + q["name"], [128, NTT, N_EXP]); Rgt_ = Res()
                    load("sp", gt_[:], q["GATE"].rearrange("(t p) e -> p t e", p=128), R=[q["R"]["GATE"]], W=[Rgt_])
                    for ti in range(NTT):
                        a = ai % 2
                        ai += 1
                        for ex in range(N_EXP):
                            b = gi % 4
                            gi += 1
                            S.dma("pool", (lambda o_, ia, i_, cb: lambda e: e.indirect_dma_start(
                                out=o_, out_offset=None, in_=i_, in_offset=bass.IndirectOffsetOnAxis(ap=ia, axis=0),
                                bounds_check=S.regs[cb], oob_is_err=False))(gb[b][:], ix[:, ti * N_EXP + ex: ti * N_EXP + ex + 1], q["Y"][ex][:, :], cap - 1),
                                reads=[q["RY"][ex], Rix], writes=[Rgb[b]])
                            if ex == 0:
                                ts("dve", acc[a][:], gb[b][:], gt_[:, ti, ex:ex + 1], None, ALU.mult, None, [Rgb[b], Rgt_], [Racc_[a]])
                            else:
                                stt("dve", acc[a][:], gb[b][:], gt_[:, ti, ex:ex + 1], acc[a][:], ALU.mult, ALU.add, [Rgb[b], Rgt_, Racc_[a]], [Racc_[a]])
                        store("act", q["ACC"][ti * 128:(ti + 1) * 128, :], acc[a][:], R=[Racc_[a]], W=[q["RACC"][ti]])
            S.barrier()

            for q in act_seqs:
                Tq = q["T"]
                j = q["j"]
                with ExitStack() as st:
                    RW = Res()
                    g2r = sb(st, "g2r", [128, D]); load("sp", g2r[:], MOD[l, j:j + 1, 5 * D:6 * D].broadcast_to([128, D]), R=[RMOD], W=[RW])
                    fnr = sb(st, "fnr", [128, D]); load("sp", fnr[:], fng.rearrange("(o d) -> o d", o=1).broadcast_to([128, D]), W=[RW])
                    xa = [sb(st, "x7a%d" % i, [128, D]) for i in range(2)]; Rxa = [Res(), Res()]
                    fa = [sb(st, "f7a%d" % i, [128, D]) for i in range(2)]; Rfa = [Res(), Res()]
                    junk = sb(st, "junk7", [128, D]); Rjunk = Res()
                    ss = sb(st, "ss7", [128, 1]); Rss = Res()
                    for ti in range(Tq // 128):
                        i = ti % 2
                        tok0 = ti * 128
                        load("sp", xa[i][:], q["XB"][tok0:tok0 + 128, :], R=[q["R"]["XB"]], W=[Rxa[i]])
                        load("sp", fa[i][:], q["ACC"][tok0:tok0 + 128, :], R=[q["RACC"][ti]], W=[Rfa[i]])
                        tt("dve", fa[i][:], fa[i][:], g2r[:], ALU.mult, [Rfa[i], RW], [Rfa[i]])
                        tt("dve", xa[i][:], xa[i][:], fa[i][:], ALU.add, [Rxa[i], Rfa[i]], [Rxa[i]])
                        if last:
                            act(junk[:], xa[i][:], AF.Square, [Rxa[i]], [Rjunk, Rss], accum_out=ss[:, 0:1])
                            rsqrt_ip("dve", ss[:], 1.0 / D, Rss)
                            stt("dve", xa[i][:], xa[i][:], ss[:, 0:1], fnr[:], ALU.mult, ALU.mult, [Rxa[i], Rss, RW], [Rxa[i]])
                            store("act", out[q["s"], tok0:tok0 + 128, :], xa[i][:], R=[Rxa[i]], W=[Res()])
                        else:
                            store("act", q["XA"][tok0:tok0 + 128, :], xa[i][:], R=[Rxa[i]], W=[q["R"]["XA"]])
                S.barrier()

        S.emit(top)
    return nc, S


_ROPE_PERM = np.array(list(range(8, 16)) + list(range(0, 8)) + list(range(24, 32)) + list(range(16, 24)))


def prep_shared(inp, T, TC, L):
    f = lambda a: np.ascontiguousarray(np.asarray(a, dtype=np.float32))
    inp = {kk: (np.asarray(v)[:L] if kk not in ("x", "c", "ctx", "c_ctx", "final_norm_g") else v) for kk, v in inp.items()}
    sh = {}
    w_in = f(inp["w_in"])
    sh["mod_w"] = f(inp["mod_w"]); sh["mod_b"] = f(inp["mod_b"])
    sh["n1gT"] = vecT(f(inp["norm1_g"])); sh["n2gT"] = vecT(f(inp["norm2_g"])); sh["n2g"] = f(inp["norm2_g"])
    sh["w_in"] = w_in
    sh["w_krs"] = np.ascontiguousarray(np.concatenate([np.zeros((L, D, 64), np.float32), w_in[:, :, OFF_KR + _ROPE_PERM]], axis=-1))
    sh["qagT"] = vecT(f(inp["q_a_g"])); sh["kvagT"] = vecT(f(inp["kv_a_g"]))
    wqb = f(inp["w_q_b"]).reshape(L, 256, NH, 96)
    sh["wq"] = np.ascontiguousarray(wqb.reshape(L, 256, NH * 96))
    sh["wqs"] = np.ascontiguousarray(np.concatenate([np.zeros((L, 256, NH, 64), np.float32), wqb[..., 64:96][..., _ROPE_PERM]], axis=-1).reshape(L, 256, NH * 96))
    wkv = f(inp["w_kv_b"]).reshape(L, 128, NH, 128)
    sh["wk"] = np.ascontiguousarray(wkv[..., 0:64].reshape(L, 128, NH * 64))
    sh["wv"] = np.ascontiguousarray(wkv[..., 64:128].reshape(L, 128, NH * 64))
    sh["dwwT"] = np.ascontiguousarray(f(inp["conv_dw_w"]).transpose(0, 2, 1).reshape(L, 2, 128, CONV_K).transpose(0, 2, 1, 3))
    sh["dwbT"] = vecT(f(inp["conv_dw_b"])); sh["clngT"] = vecT(f(inp["conv_ln_g"])); sh["clnbT"] = vecT(f(inp["conv_ln_b"]))
    sh["hsw"] = f(inp["hy_short_w"]); sh["hsb"] = f(inp["hy_short_b"])
    sh["hw1"] = f(inp["hy_w1"]); sh["hb1T"] = f(inp["hy_b1"])[..., None]
    sh["hw2"] = f(inp["hy_w2"]); sh["hb2T"] = f(inp["hy_b2"])[..., None]
    sh["hw3"] = f(inp["hy_w3"]); sh["hfrT"] = np.ascontiguousarray(f(inp["hy_sin_freq"]).transpose(0, 2, 1))
    sh["hdec"] = f(inp["hy_decay"]); sh["hskip"] = f(inp["hy_skip"])
    gnv = f(inp["group_norm_g"])
    sh["gnT"] = vecT(gnv); sh["gnaT"] = np.ascontiguousarray(gnv[:, 0:512].reshape(L, NH, 64).transpose(0, 2, 1)); sh["gn"] = gnv
    sh["w_out"] = f(inp["w_out"]); sh["router"] = f(inp["router_w"])
    sh["w_gate"] = f(inp["w_gate"]); sh["w_up"] = f(inp["w_up"]); sh["w_down"] = f(inp["w_down"])
    sh["fng"] = f(inp["final_norm_g"])
    sh["ident_f"] = np.eye(128, dtype=np.float32); sh["ident_b"] = _bf(np.eye(128, dtype=np.float32))
    sh["triu"] = np.ascontiguousarray(np.triu(np.ones((128, 128), np.float32), 1))
    rc, rs = rope_tables(T)
    sh["ropeC"] = rc; sh["ropeS"] = rs
    sh["sgn"] = (1.0 - 2.0 * (np.arange(128) % 2)).astype(np.float32)[:, None]
    for nm, TTT in (("L", T), ("C", TC)):
        hc = hyena_consts(TTT)
        for k in ("zT", "tposT", "maskT", "FW", "IV"):
            sh["hy_%s_%s" % (k, nm)] = hc[k]
    return sh


_CACHE = {}


def run(inp, n_cores, T, TC, L, NS=2, dbg=()):
    key = (T, TC, L, NS, tuple(dbg))
    import time as _t
    t0 = _t.time()
    if key not in _CACHE:
        _CACHE[key] = build_program(T, TC, L, NS, dbg)
    print("build %.1fs" % (_t.time() - t0))
    nc, S = _CACHE[key]
    sh = prep_shared(inp, T, TC, L)
    x = np.asarray(inp["x"], dtype=np.float32); ctx = np.asarray(inp["ctx"], dtype=np.float32)
    c = np.asarray(inp["c"], dtype=np.float32); cc = np.asarray(inp["c_ctx"], dtype=np.float32)
    in_maps = []
    for i in range(n_cores):
        m = dict(sh)
        m["x"] = np.ascontiguousarray(x[i * NS:(i + 1) * NS]); m["ctx"] = np.ascontiguousarray(ctx[i * NS:(i + 1) * NS])
        c3 = np.concatenate([c[i * NS:(i + 1) * NS], cc[None, :]], axis=0)
        m["c3T"] = np.ascontiguousarray(c3.reshape(3, D // 128, 128).transpose(2, 1, 0))
        in_maps.append(m)
    import time as _t
    t0 = _t.time()
    res = run_bass_kernel_spmd(nc, in_maps, core_ids=list(range(n_cores)))
    print("spmd launch wall %.1fs" % (_t.time() - t0))
    return res


def kernel(**inputs):
    res = run(inputs, 8, 4096, 256, 2)
    return np.concatenate([np.asarray(r["out"]) for r in res.results], axis=0).astype(np.float32)
```
